# Optimizing a Trainium2 kernel written in Bass

```python
import math
import jax, jax.numpy as jnp
from jax import lax
import numpy as np

D_MODEL = 2048
BATCH = 2
SEQ = 4096
DEPTH = 1

MEM_LEN = 256
CONV_WIDTH = D_MODEL // 2
CONV_GROUPS = 8
CONV_KERNEL = 31
DIFF_HEADS = 8
DIFF_HEAD_DIM = (D_MODEL - CONV_WIDTH) // DIFF_HEADS // 2
DIFF_V_DIM = 2 * DIFF_HEAD_DIM
ATTN_QK_WIDTH = DIFF_HEADS * 2 * DIFF_HEAD_DIM
ATTN_V_WIDTH = DIFF_HEADS * DIFF_V_DIM
MIX_WIDTH = CONV_WIDTH + ATTN_V_WIDTH
IN_WIDTH = 2 * CONV_WIDTH + 2 * ATTN_QK_WIDTH + ATTN_V_WIDTH
Q_BLOCK = 128
CROSS_HEADS = 4
CROSS_HEAD_DIM = D_MODEL // CROSS_HEADS
PEER_HEADS = 8
PEER_N_KEYS = 128
PEER_N_EXPERTS = PEER_N_KEYS * PEER_N_KEYS
PEER_TOPK = 16
PEER_KEY_DIM = 256
PEER_HALF = PEER_KEY_DIM // 2
PEER_CHUNK = 128
RMS_EPS = 1e-6
LN_EPS = 1e-5
NEG_INF = -1e30

kernel_name = "hybrid_conv_diffattn_peer_block"


def rmsnorm(x, g):
    xf = x.astype(jnp.float32)
    y = xf * lax.rsqrt(jnp.mean(xf * xf, axis=-1, keepdims=True) + RMS_EPS)
    return (y * g.astype(jnp.float32)).astype(x.dtype)


def lambda_init_for(layer_idx):
    return 0.8 - 0.6 * math.exp(-0.3 * layer_idx)


def conformer_conv_group(z, w_dw, b_dw, ln_g, ln_b):
    B, S, _ = z.shape
    a, gate = jnp.split(z, 2, axis=-1)
    u = a * jax.nn.sigmoid(gate)
    y = lax.conv_general_dilated(
        u, w_dw[:, None, :].astype(u.dtype), window_strides=(1,),
        padding=[(CONV_KERNEL - 1, 0)],
        dimension_numbers=('NWC', 'WIO', 'NWC'),
        feature_group_count=CONV_WIDTH) + b_dw
    yf = y.astype(jnp.float32).reshape(B, S, CONV_GROUPS, CONV_WIDTH // CONV_GROUPS)
    mu = jnp.mean(yf, axis=-1, keepdims=True)
    var = jnp.mean(jnp.square(yf - mu), axis=-1, keepdims=True)
    yf = ((yf - mu) * lax.rsqrt(var + LN_EPS)).reshape(B, S, CONV_WIDTH)
    yf = yf * ln_g.astype(jnp.float32) + ln_b.astype(jnp.float32)
    return jax.nn.silu(yf).astype(z.dtype)


def diff_attention_group(q, k, v, lam, subln_g, lam_init):
    B, S = q.shape[0], q.shape[1]
    nb = S // Q_BLOCK
    scale = DIFF_HEAD_DIM ** -0.5
    k_pos = jnp.arange(S)

    def block(i):
        qb = lax.dynamic_slice_in_dim(q, i * Q_BLOCK, Q_BLOCK, axis=1)
        s = jnp.einsum('bqhcd,bkhcd->bchqk', qb, k).astype(jnp.float32) * scale
        q_pos = i * Q_BLOCK + jnp.arange(Q_BLOCK)
        mask = k_pos[None, :] <= q_pos[:, None]
        p = jax.nn.softmax(jnp.where(mask, s, NEG_INF), axis=-1)
        a = p[:, 0] - lam * p[:, 1]
        o = jnp.einsum('bhqk,bkhd->bqhd', a.astype(v.dtype), v)
        of = o.astype(jnp.float32)
        of = of * lax.rsqrt(jnp.mean(of * of, axis=-1, keepdims=True) + RMS_EPS)
        of = of * subln_g.astype(jnp.float32) * (1.0 - lam_init)
        return of.astype(v.dtype)

    out = lax.map(block, jnp.arange(nb))
    return out.transpose(1, 0, 2, 3, 4).reshape(B, S, ATTN_V_WIDTH)


def memory_cross_attention(hn, mem, mem_g, w_cq, w_ckv, w_co):
    B, S, D = hn.shape
    mn = rmsnorm(mem, mem_g)
    q = (hn @ w_cq).reshape(B, S, CROSS_HEADS, CROSS_HEAD_DIM)
    kv = (mn @ w_ckv).reshape(B, mem.shape[1], 2, CROSS_HEADS, CROSS_HEAD_DIM)
    k, v = kv[:, :, 0], kv[:, :, 1]
    s = jnp.einsum('bqhd,bkhd->bhqk', q, k).astype(jnp.float32) * (CROSS_HEAD_DIM ** -0.5)
    p = jax.nn.softmax(s, axis=-1).astype(v.dtype)
    o = jnp.einsum('bhqk,bkhd->bqhd', p, v).reshape(B, S, D)
    return o @ w_co


def peer_ffn(xn, w_pq, keys1, keys2, u_tab, v_tab):
    B, S, D = xn.shape
    T = B * S
    xt = xn.reshape(T, D)
    q = (xt @ w_pq).reshape(T, PEER_HEADS, 2, PEER_HALF)
    s1 = jnp.einsum('thd,hnd->thn', q[:, :, 0], keys1).astype(jnp.float32)
    s2 = jnp.einsum('thd,hnd->thn', q[:, :, 1], keys2).astype(jnp.float32)
    v1, i1 = lax.top_k(s1, PEER_TOPK)
    v2, i2 = lax.top_k(s2, PEER_TOPK)
    cand = (v1[..., :, None] + v2[..., None, :]).reshape(T, PEER_HEADS, PEER_TOPK * PEER_TOPK)
    sc, ci = lax.top_k(cand, PEER_TOPK)
    e1 = jnp.take_along_axis(i1, ci // PEER_TOPK, axis=-1)
    e2 = jnp.take_along_axis(i2, ci % PEER_TOPK, axis=-1)
    ids = (e1 * PEER_N_KEYS + e2).reshape(T, PEER_HEADS * PEER_TOPK)
    g = jax.nn.softmax(sc, axis=-1).reshape(T, PEER_HEADS * PEER_TOPK)

    def chunk(args):
        xc, idc, gc = args
        uc = jnp.take(u_tab, idc, axis=0)
        h = jnp.einsum('cd,ckd->ck', xc, uc)
        w = gc.astype(xc.dtype) * jax.nn.gelu(h, approximate=False)
        return jnp.einsum('ck,ckd->cd', w, jnp.take(v_tab, idc, axis=0))

    nc = T // PEER_CHUNK
    y = lax.map(chunk, (xt.reshape(nc, PEER_CHUNK, D),
                        ids.reshape(nc, PEER_CHUNK, -1),
                        g.reshape(nc, PEER_CHUNK, -1)))
    return y.reshape(B, S, D)


def setup_inputs(seed: int = 0) -> dict:
    key = jax.random.key(seed)
    ks = iter(jax.random.split(key, 32))
    L, D = DEPTH, D_MODEL
    f32 = jnp.float32

    def nrm(shape, scale):
        return jax.random.normal(next(ks), shape, f32) * scale

    def gain(shape):
        return 1.0 + 0.02 * jax.random.normal(next(ks), shape, f32)

    return {
        "x": nrm((BATCH, SEQ, D), 1.0),
        "mem": nrm((BATCH, MEM_LEN, D), 1.0),
        "norm_mix_g": gain((L, D)),
        "w_in": nrm((L, D, IN_WIDTH), D ** -0.5),
        "conv_dw_w": nrm((L, CONV_KERNEL, CONV_WIDTH), CONV_KERNEL ** -0.5),
        "conv_dw_b": nrm((L, CONV_WIDTH), 0.02),
        "conv_ln_g": gain((L, CONV_WIDTH)),
        "conv_ln_b": nrm((L, CONV_WIDTH), 0.02),
        "lambda_q1": nrm((L, DIFF_HEAD_DIM), 0.1),
        "lambda_k1": nrm((L, DIFF_HEAD_DIM), 0.1),
        "lambda_q2": nrm((L, DIFF_HEAD_DIM), 0.1),
        "lambda_k2": nrm((L, DIFF_HEAD_DIM), 0.1),
        "diff_subln_g": gain((L, DIFF_V_DIM)),
        "w_out": nrm((L, MIX_WIDTH, D), MIX_WIDTH ** -0.5),
        "norm_cross_g": gain((L, D)),
        "norm_mem_g": gain((L, D)),
        "w_cq": nrm((L, D, D), D ** -0.5),
        "w_ckv": nrm((L, D, 2 * D), D ** -0.5),
        "w_co": nrm((L, D, D), D ** -0.5),
        "norm_peer_g": gain((L, D)),
        "w_pq": nrm((L, D, PEER_HEADS * PEER_KEY_DIM), D ** -0.5),
        "peer_keys1": nrm((L, PEER_HEADS, PEER_N_KEYS, PEER_HALF), PEER_HALF ** -0.5),
        "peer_keys2": nrm((L, PEER_HEADS, PEER_N_KEYS, PEER_HALF), PEER_HALF ** -0.5),
        "peer_u": nrm((L, PEER_N_EXPERTS, D), D ** -0.5),
        "peer_v": nrm((L, PEER_N_EXPERTS, D), PEER_HEADS ** -0.5),
        "final_norm_g": gain((D,)),
    }


def reference(x, mem, norm_mix_g, w_in, conv_dw_w, conv_dw_b, conv_ln_g, conv_ln_b,
              lambda_q1, lambda_k1, lambda_q2, lambda_k2, diff_subln_g, w_out,
              norm_cross_g, norm_mem_g, w_cq, w_ckv, w_co,
              norm_peer_g, w_pq, peer_keys1, peer_keys2, peer_u, peer_v, final_norm_g):
    B, S, D = x.shape
    h = x
    for l in range(DEPTH):
        hn = rmsnorm(h, norm_mix_g[l])
        z = hn @ w_in[l]
        o0 = 2 * CONV_WIDTH
        o1 = o0 + ATTN_QK_WIDTH
        o2 = o1 + ATTN_QK_WIDTH
        conv_out = conformer_conv_group(z[..., :o0], conv_dw_w[l], conv_dw_b[l],
                                        conv_ln_g[l], conv_ln_b[l])
        q = z[..., o0:o1].reshape(B, S, DIFF_HEADS, 2, DIFF_HEAD_DIM)
        k = z[..., o1:o2].reshape(B, S, DIFF_HEADS, 2, DIFF_HEAD_DIM)
        v = z[..., o2:].reshape(B, S, DIFF_HEADS, DIFF_V_DIM)
        lam_init = lambda_init_for(l)
        lam = (jnp.exp(jnp.sum(lambda_q1[l].astype(jnp.float32) * lambda_k1[l].astype(jnp.float32)))
               - jnp.exp(jnp.sum(lambda_q2[l].astype(jnp.float32) * lambda_k2[l].astype(jnp.float32)))
               + lam_init)
        attn_out = diff_attention_group(q, k, v, lam, diff_subln_g[l], lam_init)
        mix = jnp.concatenate([conv_out, attn_out], axis=-1)
        h = h + mix @ w_out[l]
        h = h + memory_cross_attention(rmsnorm(h, norm_cross_g[l]), mem, norm_mem_g[l],
                                       w_cq[l], w_ckv[l], w_co[l])
        h = h + peer_ffn(rmsnorm(h, norm_peer_g[l]), w_pq[l], peer_keys1[l], peer_keys2[l],
                         peer_u[l], peer_v[l])
    return rmsnorm(h, final_norm_g)
```

```python
import numpy as np
from contextlib import ExitStack
import concourse.bass as bass
import concourse.mybir as mybir
from concourse.bass_utils import run_bass_kernel_spmd

F32, BF16 = mybir.dt.float32, mybir.dt.bfloat16
AF = mybir.ActivationFunctionType
ALU = mybir.AluOpType
AX = mybir.AxisListType

D = 2048
NT = 8
NW = 32
NCH = 16
RMS_EPS = 1e-6
LN_EPS = 1e-5
LAM_INIT = 0.8 - 0.6 * 1.0
DBG = False
DBG_MEM = False


class Key:
    __slots__ = ("name", "w", "r", "sem", "tot")

    def __init__(self, name):
        self.name = name
        self.w = None
        self.r = {}
        self.sem = None
        self.tot = 0


class Sched:
    ENG = ("pe", "act", "dve", "pool", "sp")

    def __init__(self, nc, es):
        self.nc = nc
        self.es = es
        self.eng = {"pe": nc.tensor, "act": nc.scalar, "dve": nc.vector, "pool": nc.gpsimd, "sp": nc.sync}
        self.semobj = {}
        for e in self.ENG:
            self.semobj[e] = es.enter_context(nc.semaphore("s_" + e))
        self.cnt = {e: 0 for e in self.ENG}
        self.waited = {e: {} for e in self.ENG}
        self.clock = {}
        self.dkeys = []

    def key(self, name):
        return Key(name)

    def _need(self, eng, ev):
        sk, val = ev
        w = self.waited[eng]
        if w.get(sk, 0) >= val:
            return
        self.eng[eng].wait_ge(self.semobj[sk], val)
        w[sk] = val
        snap = self.clock.get(ev)
        if snap:
            for k2, v2 in snap.items():
                if w.get(k2, 0) < v2:
                    w[k2] = v2

    def _deps(self, eng, r, w, dma=False):
        for k in r:
            if k.w is not None and not (eng == "pe" and k.w[0] == "pe"):
                self._need(eng, k.w)
        for k in w:
            if k.w is not None:
                if eng == "pe" and k.w[0] == "pe":
                    pass
                elif dma and k.sem is not None and k.w[0] == k.sem:
                    pass
                else:
                    self._need(eng, k.w)
            for sk, val in k.r.items():
                if sk == eng and eng == "pe":
                    continue
                self._need(eng, (sk, val))

    def op(self, eng, fn, r=(), w=()):
        self._deps(eng, r, w)
        inst = fn()
        inst.then_inc(self.semobj[eng], 1)
        self.cnt[eng] += 1
        ev = (eng, self.cnt[eng])
        snap = dict(self.waited[eng])
        snap[eng] = self.cnt[eng]
        self.clock[ev] = snap
        for k in r:
            if k.r.get(eng, 0) < ev[1]:
                k.r[eng] = ev[1]
        for k in w:
            k.w = ev
            k.r = {}
        return inst

    def dma(self, q, out, in_, r, w):
        (wk,) = w
        if wk.sem is None:
            name = "d%d" % len(self.dkeys)
            self.semobj[name] = self.es.enter_context(self.nc.semaphore("s_" + name))
            wk.sem = name
            self.dkeys.append(wk)
        self._deps(q, r, w, dma=True)
        inst = self.eng[q].dma_start(out=out, in_=in_)
        inst.then_inc(self.semobj[wk.sem], 16)
        wk.tot += 16
        ev = (wk.sem, wk.tot)
        self.clock[ev] = dict(self.waited[q])
        for k in r:
            k.r[wk.sem] = wk.tot
        wk.w = ev
        wk.r = {}
        return inst

    def barrier(self):
        for e in self.ENG:
            for e2 in ("pe", "act", "dve", "pool"):
                if self.cnt[e2] > 0:
                    self._need(e, (e2, self.cnt[e2]))
            for k in self.dkeys:
                if k.tot > 0:
                    self._need(e, (k.sem, k.tot))


def build():
    nc = bass.Bass("TRN2", target_bir_lowering=False)

    def din(name, shape):
        return nc.dram_tensor(name, shape, F32, kind="ExternalInput").ap()

    xw = din("xw", [NW * 128, D])
    valid = din("valid", [128, NW])
    memb = din("memb", [256, D])
    norm_mix_g = din("norm_mix_g", [D])
    w_in = din("w_in", [D, 5120])
    conv_dw_w = din("conv_dw_w", [31, 1024])
    conv_dw_b = din("conv_dw_b", [1024])
    conv_ln_g = din("conv_ln_g", [1024])
    conv_ln_b = din("conv_ln_b", [1024])
    lq1 = din("lambda_q1", [64])
    lk1 = din("lambda_k1", [64])
    lq2 = din("lambda_q2", [64])
    lk2 = din("lambda_k2", [64])
    subln = din("diff_subln_g", [128])
    w_out = din("w_out", [D, D])
    norm_cross_g = din("norm_cross_g", [D])
    norm_mem_g = din("norm_mem_g", [D])
    w_cq = din("w_cq", [D, D])
    w_ckv = din("w_ckv", [D, 2 * D])
    w_co = din("w_co", [D, D])
    norm_peer_g = din("norm_peer_g", [D])
    w_pq = din("w_pq", [D, D])
    keys1 = din("peer_keys1", [8, 128, 128])
    keys2 = din("peer_keys2", [8, 128, 128])
    peer_u = din("peer_u", [16384, D])
    peer_v = din("peer_v", [16384, D])
    final_g = din("final_norm_g", [D])
    y_out = nc.dram_tensor("y", [NT * 128, D], F32, kind="ExternalOutput").ap()
    wd = nc.dram_tensor("wd_scratch", [128, 128, NT * 128], BF16, kind="Internal").ap()
    sdram = nc.dram_tensor("s_scratch", [NT, 128, 2, 8, 128], F32, kind="Internal").ap()
    dbg = None
    if DBG:
        dbg = nc.dram_tensor("dbg", [3, NT * 128, D], F32, kind="ExternalOutput").ap()

    with ExitStack() as es:
        S = Sched(nc, es)
        V, A, G, PE = nc.vector, nc.scalar, nc.gpsimd, nc.tensor

        SB_BASE, SB_TOP = 16512, 229344
        mem = {"ptr": SB_BASE, "n": 0, "limit": SB_TOP}

        class Scope:
            def __enter__(self):
                self.mark = mem["ptr"]
                return self

            def __exit__(self, *a):
                mem["ptr"] = self.mark
                return False

        def sb(stack, name, shape, dtype, limit=None):
            nbytes = int(np.prod(shape[1:])) * (4 if dtype == F32 else 2)
            nbytes = (nbytes + 63) // 64 * 64
            off = mem["ptr"]
            lim = mem["limit"]
            assert off + nbytes <= lim, ("SBUF overflow", name, off, nbytes, lim)
            mem["ptr"] = off + nbytes
            mem["n"] += 1
            return nc.alloc_sbuf_tensor_at("%s_%d" % (name, mem["n"]), list(shape), dtype, offset=off)

        H_OFF = SB_TOP - NT * D * 4

        def bc(ap1d, n):
            return ap1d.rearrange("(o n) -> o n", o=1).to_broadcast([128, n])

        PS = [es.enter_context(nc.psum_tensor("ps%d" % i, [128, 512], F32)) for i in range(6)]
        PSK = [S.key("ps%d" % i) for i in range(6)]
        TP = [es.enter_context(nc.psum_tensor("tp%d" % i, [128, 1024], BF16)) for i in range(2)]
        TPK = [S.key("tp%d" % i) for i in range(2)]

        identf = sb(es, "identf", [128, 128], F32)
        ident = sb(es, "ident", [128, 128], BF16)
        tri = sb(es, "tri", [128, 128], BF16)
        zer = sb(es, "zer", [128, 512], BF16)
        onesb = sb(es, "onesb", [128, 128], BF16)
        onesf = sb(es, "onesf", [128, 128], F32)
        epsr = sb(es, "epsr", [128, 1], F32)
        epsl = sb(es, "epsl", [128, 1], F32)
        kc = S.key("consts")
        S.op("pool", lambda: G.memset(identf[:], 0.0), w=[kc])
        S.op("pool", lambda: G.affine_select(out=identf[:], in_=identf[:], pattern=[[-1, 128]], compare_op=ALU.not_equal,
                                             fill=1.0, base=0, channel_multiplier=1), r=[kc], w=[kc])
        S.op("dve", lambda: V.tensor_copy(out=ident[:], in_=identf[:]), r=[kc], w=[kc])
        S.op("pool", lambda: G.memset(onesf[:], 1.0), w=[kc])
        S.op("pool", lambda: G.affine_select(out=onesf[:], in_=onesf[:], pattern=[[1, 128]], compare_op=ALU.is_ge,
                                             fill=0.0, base=0, channel_multiplier=-1), r=[kc], w=[kc])
        S.op("dve", lambda: V.tensor_copy(out=tri[:], in_=onesf[:]), r=[kc], w=[kc])
        S.op("dve", lambda: V.memset(onesf[:], 1.0 / 128.0), r=[kc], w=[kc])
        S.op("dve", lambda: V.memset(zer[:], 0.0), w=[kc])
        S.op("dve", lambda: V.memset(onesb[:], 1.0), w=[kc])
        S.op("dve", lambda: V.memset(epsr[:], RMS_EPS), w=[kc])
        S.op("dve", lambda: V.memset(epsl[:], LN_EPS), w=[kc])

        sm_i = [0]

        def rms_A(src, srck, gbc, gk, hn, hnk, ss, ssk):
            S.op("act", lambda: A.activation(out=hn[:], in_=src, func=AF.Square, accum_out=ss[:, 0:1]), r=[srck], w=[hnk, ssk])
            S.op("dve", lambda: V.tensor_scalar(out=ss[:, 1:2], in0=ss[:, 0:1], scalar1=1.0 / D, scalar2=RMS_EPS, op0=ALU.mult, op1=ALU.add), r=[ssk], w=[ssk])
            S.op("act", lambda: A.activation(out=ss[:, 2:3], in_=ss[:, 1:2], func=AF.Sqrt), r=[ssk], w=[ssk])
            S.op("dve", lambda: V.reciprocal(out=ss[:, 3:4], in_=ss[:, 2:3]), r=[ssk], w=[ssk])
            S.op("dve", lambda: V.scalar_tensor_tensor(out=hn[:], in0=src, scalar=ss[:, 3:4], in1=gbc[:], op0=ALU.mult, op1=ALU.mult), r=[srck, ssk, gk], w=[hnk])

        def rms_B(hn, hnk, dstT, dstk, col0, evac_alt=0):
            for half in range(2):
                for c in range(8):
                    S.op("pe", lambda c=c: PE.transpose(out=TP[half][:, c * 128:(c + 1) * 128], in_=hn[:, (half * 8 + c) * 128:(half * 8 + c + 1) * 128], identity=ident[:]), r=[hnk, kc], w=[TPK[half]])
                dst = dstT[:, half * 8:(half + 1) * 8, col0:col0 + 128]
                srcp = TP[half][:, :].rearrange("p (c n) -> p c n", c=8)
                if (half + evac_alt) % 2 == 0:
                    S.op("act", lambda: A.copy(out=dst, in_=srcp), r=[TPK[half]], w=[dstk])
                else:
                    S.op("dve", lambda: V.tensor_copy(out=dst, in_=srcp), r=[TPK[half]], w=[dstk])

        def rms_to_T(st, src, srck, gbc, gk, hn, hnk, sqj, ss, ssk, dstT, dstk, col0, evac_alt=0):
            rms_A(src, srck, gbc, gk, hn, hnk, ss, ssk)
            rms_B(hn, hnk, dstT, dstk, col0, evac_alt)

        def rms_loop(n, src_fn, gbc, gk, hn2, hn2k, ss2, ss2k, dstT, dstk):
            a0, k0 = src_fn(0)
            rms_A(a0, k0, gbc, gk, hn2[0], hn2k[0], ss2[0], ss2k[0])
            for i in range(n):
                if i + 1 < n:
                    a1, k1 = src_fn(i + 1)
                    rms_A(a1, k1, gbc, gk, hn2[(i + 1) % 2], hn2k[(i + 1) % 2], ss2[(i + 1) % 2], ss2k[(i + 1) % 2])
                rms_B(hn2[i % 2], hn2k[i % 2], dstT, dstk, i * 128, evac_alt=i)

        def wblock(Wt, Wk, wdram, c0, ncols=512):
            S.dma("pool", Wt[:, :, 0:ncols], wdram[:, c0:c0 + ncols].rearrange("(c p) n -> p c n", p=128), r=[], w=[Wk])

        ps_rr = [0]

        def next_ps():
            i = ps_rr[0] % 6
            ps_rr[0] += 1
            return PS[i], PSK[i]

        evac_rr = [0]

        def evac(out, in_, r, w):
            evac_rr[0] += 1
            if evac_rr[0] % 2 == 0:
                S.op("act", lambda: A.copy(out=out, in_=in_), r=r, w=w)
            else:
                S.op("dve", lambda: V.tensor_copy(out=out, in_=in_), r=r, w=w)

        def proj_featT(Wt, Wk, wc0, actT, actk, t0, n, dst, dstk):
            ps, pk = next_ps()
            for c in range(NCH):
                S.op("pe", lambda c=c: PE.matmul(ps[:, 0:n], lhsT=Wt[:, c, wc0:wc0 + 128], rhs=actT[:, c, t0:t0 + n], start=(c == 0), stop=(c == NCH - 1)), r=[Wk, actk], w=[pk])
            return ps, pk

        def proj_tok(Wt, Wk, ncols, actT, actk, t0):
            ps, pk = next_ps()
            for c in range(NCH):
                S.op("pe", lambda c=c: PE.matmul(ps[:, 0:ncols], lhsT=actT[:, c, t0:t0 + 128], rhs=Wt[:, c, 0:ncols], start=(c == 0), stop=(c == NCH - 1)), r=[Wk, actk], w=[pk])
            return ps, pk

        mark_mix = None
        attnT = sb(es, "attnT", [128, 8, NT * 128], BF16)
        attnTk = S.key("attnT")
        mark_mix = mem["ptr"] - 8 * NT * 128 * 2

        with Scope() as st:
            KT = sb(st, "KT", [128, 4, NW * 128], BF16)
            VA = sb(st, "VA", [128, NW, 4, 128], BF16)
            QT = sb(st, "QT", [128, 4, NT * 128], BF16)
            KTk, VAk, QTk = S.key("KT"), S.key("VA"), S.key("QT")
            hnT = [sb(st, "hnT%d" % i, [128, NCH, 256], BF16) for i in range(2)]
            hnTk = [S.key("hnT%d" % i) for i in range(2)]
            Wb = [sb(st, "Wb%d" % i, [128, NCH, 512], BF16) for i in range(3)]
            Wbk = [S.key("Wb%d" % i) for i in range(3)]
            xs_off = mem["ptr"]
            xs = [sb(st, "xs%d" % i, [128, D], F32) for i in range(2)]
            xsk = [S.key("xs%d" % i) for i in range(2)]
            hn2 = [sb(st, "hn%d" % i, [128, D], BF16) for i in range(2)]
            hn2k = [S.key("hn%d" % i) for i in range(2)]
            gmix = sb(st, "gmix", [128, D], F32)
            gmk = S.key("gmix")
            ss = [sb(st, "ss%d" % i, [128, 4], F32) for i in range(2)]
            ssk = [S.key("ss%d" % i) for i in range(2)]
            NPT = 6
            PT = [sb(st, "PT%d" % i, [128, 512], BF16) for i in range(NPT)]
            PTk = [S.key("PT%d" % i) for i in range(NPT)]
            validt = sb(st, "validt", [128, NW], F32)
            vk = S.key("valid")
            lamt = sb(st, "lamt", [128, 4, 64], F32)
            lamk = S.key("lam")
            lams = sb(st, "lams", [128, 8], F32)
            gsub = sb(st, "gsub", [128, 128], F32)
            gsk = S.key("gsub")
            vbias = sb(st, "vbias", [128, NW], F32)
            maskA = sb(st, "maskA", [128, 2, 2, 128], BF16)
            maskB = sb(st, "maskB", [128, 2, 2, 128], BF16)
            mkk = S.key("masks")
            save_ptr = mem["ptr"]
            mem["ptr"] = xs_off
            QTbd = [sb(st, "QTbd%d" % i, [128, 4, 2, 256], BF16) for i in range(1)] * 2
            QTbdk = [S.key("QTbd0")] * 2
            Zacc = [[sb(st, "Zacc%d_%d" % (p, q), [128, 512], F32) for q in range(2)] for p in range(2)]
            Zacck = [[S.key("Zacc%d_%d" % (p, q)) for q in range(2)] for p in range(2)]
            bufA = sb(st, "bufA", [128, 512], F32)
            bufB = sb(st, "bufB", [128, 256], F32)
            bufC = sb(st, "bufC", [128, 256], F32)
            bufAk, bufBk, bufCk = S.key("bufA"), S.key("bufB"), S.key("bufC")
            assert mem["ptr"] <= xs_off + 2 * D * 4
            mem["ptr"] = save_ptr
            gsubc = sb(st, "gsubc", [128, 1], F32)
            ones1 = sb(st, "ones1", [128, 128], F32)

            if DBG_MEM:
                print("stage1 mem ptr", mem["ptr"], "limit", mem["limit"], "free", mem["limit"] - mem["ptr"])
            S.dma("sp", gmix[:], bc(norm_mix_g, D), r=[], w=[gmk])
            S.dma("sp", validt[:], valid[:, :], r=[], w=[vk])
            for i, v in enumerate((lq1, lk1, lq2, lk2)):
                S.dma("sp", lamt[:, i, :], bc(v, 64), r=[], w=[lamk])
            S.dma("sp", gsub[:], bc(subln, 128), r=[], w=[gsk])
            S.dma("sp", gsubc[:], subln.rearrange("(p o) -> p o", o=1), r=[], w=[gsk])
            S.op("dve", lambda: V.tensor_scalar(out=gsubc[:], in0=gsubc[:], scalar1=(1.0 - LAM_INIT), scalar2=None, op0=ALU.mult), r=[gsk], w=[gsk])
            S.op("dve", lambda: V.tensor_scalar(out=vbias[:], in0=validt[:], scalar1=-1.0, scalar2=30000.0, op0=ALU.add, op1=ALU.mult), r=[vk], w=[vk])
            S.op("dve", lambda: V.memset(ones1[:], 1.0), w=[mkk])
            for c_ in range(2):
                S.op("dve", lambda: V.tensor_copy(out=maskA[:, c_, 0, :], in_=tri[:]), r=[kc], w=[mkk])
                S.op("dve", lambda: V.tensor_copy(out=maskA[:, c_, 1, :], in_=onesb[:]), r=[kc], w=[mkk])
                S.op("dve", lambda: V.tensor_copy(out=maskB[:, c_, 0, :], in_=zer[:, 0:128]), r=[kc], w=[mkk])
                S.op("dve", lambda: V.tensor_copy(out=maskB[:, c_, 1, :], in_=tri[:]), r=[kc], w=[mkk])
            S.op("dve", lambda: V.tensor_tensor(out=lamt[:, 0, :], in0=lamt[:, 0, :], in1=lamt[:, 1, :], op=ALU.mult), r=[lamk], w=[lamk])
            S.op("dve", lambda: V.tensor_tensor(out=lamt[:, 2, :], in0=lamt[:, 2, :], in1=lamt[:, 3, :], op=ALU.mult), r=[lamk], w=[lamk])
            S.op("dve", lambda: V.reduce_sum(out=lams[:, 0:1], in_=lamt[:, 0, :], axis=AX.X), r=[lamk], w=[lamk])
            S.op("dve", lambda: V.reduce_sum(out=lams[:, 1:2], in_=lamt[:, 2, :], axis=AX.X), r=[lamk], w=[lamk])
            S.op("act", lambda: A.activation(out=lams[:, 2:4], in_=lams[:, 0:2], func=AF.Exp), r=[lamk], w=[lamk])
            S.op("dve", lambda: V.tensor_tensor(out=lams[:, 4:5], in0=lams[:, 2:3], in1=lams[:, 3:4], op=ALU.subtract), r=[lamk], w=[lamk])
            S.op("dve", lambda: V.tensor_scalar(out=lams[:, 5:6], in0=lams[:, 4:5], scalar1=LAM_INIT, scalar2=None, op0=ALU.add), r=[lamk], w=[lamk])
            S.op("dve", lambda: V.tensor_scalar(out=gsub[:], in0=gsub[:], scalar1=(1.0 - LAM_INIT), scalar2=None, op0=ALU.mult), r=[gsk], w=[gsk])
            lam_ap = lams[:, 5:6]

            tcount = 0
            wcount = 0
            for hh in range(2):
                qc0 = 2048 + 512 * hh
                kc0 = 3072 + 512 * hh
                vc0 = 4096 + 512 * hh
                wblock(Wb[0], Wbk[0], w_in, kc0)
                wblock(Wb[1], Wbk[1], w_in, vc0)
                wblock(Wb[2], Wbk[2], w_in, qc0)
                def stage1_A(w_):
                    u_ = w_ % 2
                    S.dma("sp", xs[u_][:], xw[w_ * 128:(w_ + 1) * 128, :], r=[], w=[xsk[u_]])
                    rms_A(xs[u_][:], xsk[u_], gmix, gmk, hn2[u_], hn2k[u_], ss[u_], ssk[u_])

                def mm_group(grp):
                    hT, hTk = hnT[grp % 2], hnTk[grp % 2]
                    for j in range(4):
                        ps, pk = proj_featT(Wb[0], Wbk[0], j * 128, hT, hTk, 0, 256, None, None)
                        evac(KT[:, j, grp * 256:(grp + 1) * 256], ps[:, 0:256], r=[pk], w=[KTk])
                    for t in range(2):
                        ps, pk = proj_tok(Wb[1], Wbk[1], 512, hT, hTk, t * 128)
                        evac(VA[:, grp * 2 + t, :, :], ps[:, 0:512].rearrange("p (h d) -> p h d", h=4), r=[pk], w=[VAk])
                    if grp >= 12:
                        for j in range(4):
                            ps, pk = proj_featT(Wb[2], Wbk[2], j * 128, hT, hTk, 0, 256, None, None)
                            evac(QT[:, j, (grp - 12) * 256:(grp - 11) * 256], ps[:, 0:256], r=[pk], w=[QTk])

                stage1_A(0)
                for grp in range(16):
                    hT, hTk = hnT[grp % 2], hnTk[grp % 2]
                    for t in range(2):
                        w_ = grp * 2 + t
                        if w_ + 1 < NW:
                            stage1_A(w_ + 1)
                        rms_B(hn2[w_ % 2], hn2k[w_ % 2], hT, hTk, t * 128, evac_alt=t)
                    if grp >= 1:
                        mm_group(grp - 1)
                mm_group(15)

                S.op("pool", lambda: G.memset(QTbd[0][:], 0.0), r=[QTk], w=[QTbdk[0]])
                TPf = [TP[0][:, :].bitcast(F32), TP[1][:, :].bitcast(F32)]
                Sbanks = [(PS[4][:, 0:512], PSK[4]), (PS[5][:, 0:512], PSK[5]), (TPf[0], TPK[0])]
                zb, zbk = TPf[1], TPK[1]
                OTb = [[PS[0], PS[1]], [PS[2], PS[3]]]
                OTbk = [[PSK[0], PSK[1]], [PSK[2], PSK[3]]]

                def norm_steps(par, hl, g):
                    steps = []
                    for qh in range(2):
                        def s1(qh=qh):
                            ot_, otk_ = OTb[par][qh], OTbk[par][qh]
                            S.op("pe", lambda: PE.matmul(zb, lhsT=ones1[:], rhs=Zacc[par][qh][:], start=True, stop=True), r=[Zacck[par][qh], mkk], w=[zbk])
                            S.op("dve", lambda: V.reciprocal(out=bufA[:], in_=zb), r=[zbk], w=[bufAk])
                            S.op("dve", lambda: V.tensor_scalar(out=bufA[:, 256:512], in0=bufA[:, 256:512], scalar1=lam_ap, scalar2=None, op0=ALU.mult), r=[bufAk, lamk], w=[bufAk])
                            S.op("dve", lambda: V.tensor_tensor(out=bufA[:], in0=ot_[:, 0:512], in1=bufA[:], op=ALU.mult), r=[otk_, bufAk], w=[bufAk])
                            S.op("dve", lambda: V.tensor_tensor(out=bufA[:, 0:256], in0=bufA[:, 0:256], in1=bufA[:, 256:512], op=ALU.subtract), r=[bufAk], w=[bufAk])
                            S.op("act", lambda: A.activation(out=bufB[:], in_=bufA[:, 0:256], func=AF.Square), r=[bufAk], w=[bufBk])

                        def s2(qh=qh):
                            S.op("pe", lambda: PE.matmul(zb[:, 0:256], lhsT=onesf[:], rhs=bufB[:], start=True, stop=True), r=[bufBk, kc], w=[zbk])
                            S.op("act", lambda: A.activation(out=bufC[:], in_=zb[:, 0:256], func=AF.Ln, bias=epsr[:, 0:1], scale=1.0), r=[zbk, kc], w=[bufCk])
                            S.op("act", lambda: A.activation(out=bufC[:], in_=bufC[:], func=AF.Exp, scale=-0.5), r=[bufCk], w=[bufCk])
                            q0 = g * 512 + qh * 256
                            S.op("dve", lambda: V.scalar_tensor_tensor(out=attnT[:, 4 * hh + hl, q0:q0 + 256], in0=bufA[:, 0:256], scalar=gsubc[:, 0:1], in1=bufC[:], op0=ALU.mult, op1=ALU.mult), r=[bufAk, bufCk, gsk], w=[attnTk])

                        steps += [s1, s2]
                    return steps

                srr = 0
                blk = 0
                pending = None
                for hl in range(4):
                    qb, qbk = QTbd[hl % 2], QTbdk[hl % 2]
                    S.op("act", lambda: A.copy(out=qb[0:64, :, 0, :], in_=QT[0:64, hl, :].rearrange("p (g q) -> p g q", g=4)), r=[QTk], w=[qbk])
                    S.op("act", lambda: A.copy(out=qb[64:128, :, 1, :], in_=QT[64:128, hl, :].rearrange("p (g q) -> p g q", g=4)), r=[QTk], w=[qbk])
                    for g in range(2):
                        par = blk % 2
                        blk += 1
                        nkt = 24 + 4 * g + 4
                        units = []
                        for kt in range(nkt):
                            idiag = kt - (24 + 4 * g)
                            for qh in range(2):
                                if idiag > 2 * qh + 1:
                                    continue
                                units.append((kt, qh, idiag))
                        LAG = 2
                        slots = {}
                        for idx in range(len(units) + LAG):
                            if idx < len(units):
                                kt, qh, idiag = units[idx]
                                sp_, spk = Sbanks[srr % 3]
                                pt, ptk = PT[srr % NPT], PTk[srr % NPT]
                                slots[idx] = (pt, ptk)
                                srr += 1
                                S.op("pe", lambda: PE.matmul(sp_, lhsT=KT[:, hl, kt * 128:(kt + 1) * 128], rhs=qb[:, 2 * g + qh, :, :].rearrange("p c q -> p (c q)"), start=True, stop=True), r=[KTk, qbk], w=[spk])
                                S.op("act", lambda: A.activation(out=pt[:], in_=sp_, func=AF.Exp, bias=vbias[:, kt:kt + 1], scale=0.125), r=[spk, vk], w=[ptk])
                                if idiag == 2 * qh:
                                    S.op("pool", lambda: G.tensor_tensor(out=pt[:], in0=pt[:], in1=maskA[:].rearrange("p c j q -> p (c j q)"), op=ALU.mult), r=[ptk, mkk], w=[ptk])
                                elif idiag == 2 * qh + 1:
                                    S.op("pool", lambda: G.tensor_tensor(out=pt[:], in0=pt[:], in1=maskB[:].rearrange("p c j q -> p (c j q)"), op=ALU.mult), r=[ptk, mkk], w=[ptk])
                                za, zak = Zacc[par][qh], Zacck[par][qh]
                                if kt == 0:
                                    S.op("dve", lambda: V.tensor_copy(out=za[:], in_=pt[:]), r=[ptk], w=[zak])
                                elif kt % 5 < 3:
                                    S.op("dve", lambda: V.tensor_tensor(out=za[:], in0=za[:], in1=pt[:], op=ALU.add), r=[ptk, zak], w=[zak])
                                else:
                                    S.op("pool", lambda: G.tensor_tensor(out=za[:], in0=za[:], in1=pt[:], op=ALU.add), r=[ptk, zak], w=[zak])
                            if idx - LAG >= 0:
                                kt, qh, idiag = units[idx - LAG]
                                pt, ptk = slots.pop(idx - LAG)
                                klast = 24 + 4 * g + 2 * qh + 1
                                S.op("pe", lambda: PE.matmul(OTb[par][qh][:, 0:512], lhsT=VA[:, kt, hl, :], rhs=pt[:], start=(kt == 0), stop=(kt == klast)), r=[ptk, VAk], w=[OTbk[par][qh]])
                            if pending and idx in (6, 14, 22, 30):
                                pending.pop(0)()
                        assert not pending
                        pending = norm_steps(par, hl, g)
                while pending:
                    pending.pop(0)()
                S._deps("sp", [], [QTbdk[0], bufAk, bufBk, bufCk] + [Zacck[p_][q_] for p_ in range(2) for q_ in range(2)], dma=True)
            S.barrier()

        convT = sb(es, "convT", [128, 8, NT * 128], BF16)
        convTk = S.key("convT")
        with Scope() as st:
            NTK = 9 * 128
            hn9 = sb(st, "hn9", [128, NCH, NTK], BF16)
            hn9k = S.key("hn9")
            uT = sb(st, "uT", [128, 8, NTK], BF16)
            uTk = S.key("uT")
            Wb = [sb(st, "cWb%d" % i, [128, NCH, 512], BF16) for i in range(2)]
            Wbk = [S.key("cWb%d" % i) for i in range(2)]
            xs = [sb(st, "cxs%d" % i, [128, D], F32) for i in range(2)]
            xsk = [S.key("cxs%d" % i) for i in range(2)]
            hn = [sb(st, "chn%d" % i, [128, D], BF16) for i in range(2)]
            hnk = [S.key("chn%d" % i) for i in range(2)]
            gmix = sb(st, "cgmix", [128, D], F32)
            gmk = S.key("cgmix")
            ss = [sb(st, "css%d" % i, [128, 4], F32) for i in range(2)]
            ssk = [S.key("css%d" % i) for i in range(2)]
            wdw_nat = sb(st, "wdw_nat", [31, 1024], F32)
            wdwT = sb(st, "wdwT", [128, 8, 31], F32)
            cpar = sb(st, "cpar", [128, 3, 8], F32)
            cpk = S.key("cpar")
            Dg2 = [sb(st, "Dg%d" % i, [128, 31, 128], BF16) for i in range(2)]
            Dg2k = [S.key("Dg%d" % i) for i in range(2)]
            sg = [sb(st, "sg%d" % i, [128, 384], F32) for i in range(2)]
            sgk = [S.key("sg%d" % i) for i in range(2)]
            yf2 = [sb(st, "yf%d" % i, [128, 512], F32) for i in range(2)]
            yc2 = [sb(st, "yc%d" % i, [128, 512], F32) for i in range(2)]
            sq2 = [sb(st, "sq%d" % i, [128, 512], F32) for i in range(2)]
            sd2 = [sb(st, "sd%d" % i, [128, 512], F32) for i in range(2)]
            yf2k = [S.key("yf%d" % i) for i in range(2)]
            yc2k = [S.key("yc%d" % i) for i in range(2)]
            sq2k = [S.key("sq%d" % i) for i in range(2)]
            sd2k = [S.key("sd%d" % i) for i in range(2)]

            S.dma("sp", gmix[:], bc(norm_mix_g, D), r=[], w=[gmk])
            S.dma("sp", wdw_nat[:], conv_dw_w[:, :], r=[], w=[cpk])
            with nc.allow_non_contiguous_dma(reason="tiny per-channel parameter vectors"):
                for i, v in enumerate((conv_dw_b, conv_ln_g, conv_ln_b)):
                    S.dma("sp", cpar[:, i, :], v.rearrange("(c p) -> p c", p=128), r=[], w=[cpk])
            for cc in range(8):
                ps, pk = next_ps()
                S.op("pe", lambda: PE.transpose(out=ps[:, 0:31], in_=wdw_nat[:, cc * 128:(cc + 1) * 128], identity=identf[0:31, 0:31]), r=[cpk, kc], w=[pk])
                evac(wdwT[:, cc, :], ps[:, 0:31], r=[pk], w=[cpk])
            def conv_src(t):
                w_ = 23 + t
                S.dma("sp", xs[t % 2][:], xw[w_ * 128:(w_ + 1) * 128, :], r=[], w=[xsk[t % 2]])
                return xs[t % 2][:], xsk[t % 2]

            rms_loop(9, conv_src, gmix, gmk, hn, hnk, ss, ssk, hn9, hn9k)
            wcount = 0
            for bl in range(2):
                Wa, Wak = Wb[0], Wbk[0]
                Wg, Wgk = Wb[1], Wbk[1]
                wblock(Wa, Wak, w_in, bl * 512)
                wblock(Wg, Wgk, w_in, 1024 + bl * 512)
                for j in range(4):
                    cc = bl * 4 + j
                    for rr in range(3):
                        t0 = rr * 384
                        psa, pka = proj_featT(Wa, Wak, j * 128, hn9, hn9k, t0, 384, None, None)
                        psg, pkg = proj_featT(Wg, Wgk, j * 128, hn9, hn9k, t0, 384, None, None)
                        s_, s_k = sg[rr % 2], sgk[rr % 2]
                        S.op("act", lambda: A.activation(out=s_[:], in_=psg[:, 0:384], func=AF.Sigmoid), r=[pkg], w=[s_k])
                        S.op("dve", lambda: V.tensor_tensor(out=uT[:, cc, t0:t0 + 384], in0=psa[:, 0:384], in1=s_[:], op=ALU.mult), r=[pka, s_k], w=[uTk])
            def build_Dg(cc):
                Dg, Dgk = Dg2[cc % 2], Dg2k[cc % 2]
                for j in range(31):
                    S.op("dve", lambda j=j: V.tensor_scalar(out=Dg[:, j, :], in0=ident[:], scalar1=wdwT[:, cc, j:j + 1], scalar2=None, op0=ALU.mult), r=[cpk, kc], w=[Dgk])

            def conv_T(u):
                cc, half = divmod(u, 2)
                Dg, Dgk = Dg2[cc % 2], Dg2k[cc % 2]
                ps, pk = next_ps()
                for j in range(31):
                    o = 128 + half * 512 - 30 + j
                    S.op("pe", lambda j=j, o=o: PE.matmul(ps[:, 0:512], lhsT=Dg[:, j, :], rhs=uT[:, cc, o:o + 512], start=(j == 0), stop=(j == 30)), r=[Dgk, uTk], w=[pk])
                if half == 0 and cc + 1 < 8:
                    build_Dg(cc + 1)
                return ps, pk

            def conv_LN(u, ps, pk):
                cc, half = divmod(u, 2)
                v_ = u % 2
                yf, yc, sq, sd = yf2[v_], yc2[v_], sq2[v_], sd2[v_]
                yfk, yck, sqk, sdk = yf2k[v_], yc2k[v_], sq2k[v_], sd2k[v_]
                S.op("act", lambda: A.activation(out=yf[:], in_=ps[:, 0:512], func=AF.Identity, bias=cpar[:, 0, cc:cc + 1], scale=1.0), r=[pk, cpk], w=[yfk])
                pm, pmk = next_ps()
                S.op("pe", lambda: PE.matmul(pm[:, 0:512], lhsT=onesf[:], rhs=yf[:], start=True, stop=True), r=[yfk, kc], w=[pmk])
                S.op("dve", lambda: V.tensor_tensor(out=yc[:], in0=yf[:], in1=pm[:, 0:512], op=ALU.subtract), r=[yfk, pmk], w=[yck])
                S.op("act", lambda: A.activation(out=sq[:], in_=yc[:], func=AF.Square), r=[yck], w=[sqk])
                pv, pvk = next_ps()
                S.op("pe", lambda: PE.matmul(pv[:, 0:512], lhsT=onesf[:], rhs=sq[:], start=True, stop=True), r=[sqk, kc], w=[pvk])
                S.op("act", lambda: A.activation(out=sd[:], in_=pv[:, 0:512], func=AF.Ln, bias=epsl[:, 0:1], scale=1.0), r=[pvk, kc], w=[sdk])
                S.op("act", lambda: A.activation(out=sd[:], in_=sd[:], func=AF.Exp, scale=-0.5), r=[sdk], w=[sdk])
                S.op("dve", lambda: V.tensor_tensor(out=yc[:], in0=yc[:], in1=sd[:], op=ALU.mult), r=[yck, sdk], w=[yck])
                S.op("act", lambda: A.activation(out=convT[:, cc, half * 512:(half + 1) * 512], in_=yc[:], func=AF.Silu, bias=cpar[:, 2, cc:cc + 1], scale=cpar[:, 1, cc:cc + 1]), r=[yck, cpk], w=[convTk])

            build_Dg(0)
            prev = conv_T(0)
            for u in range(16):
                nxt = conv_T(u + 1) if u + 1 < 16 else None
                conv_LN(u, *prev)
                prev = nxt
            S.barrier()

        h = nc.alloc_sbuf_tensor_at("h_res", [128, NT, D], F32, offset=H_OFF)
        mem["limit"] = H_OFF
        hk = [S.key("h%d" % i) for i in range(NT)]
        for i in range(NT):
            S.dma("sp", h[:, i, :], xw[(24 + i) * 128:(25 + i) * 128, :], r=[], w=[hk[i]])

        def proj_residual(st, wdram, srcs, tag, Wb=None, Wbk=None):
            if Wb is None:
                Wb = [sb(st, "%sWb%d" % (tag, i), [128, NCH, 512], BF16) for i in range(2)]
                Wbk = [S.key("%sWb%d" % (tag, i)) for i in range(2)]
            for nb in range(4):
                Wt, Wk = Wb[nb % 2], Wbk[nb % 2]
                wblock(Wt, Wk, wdram, nb * 512)
                for i in range(NT):
                    ps, pk = next_ps()
                    for c in range(NCH):
                        tns, ci, tk = srcs[c]
                        S.op("pe", lambda c=c, tns=tns, ci=ci: PE.matmul(ps[:, 0:512], lhsT=tns[:, ci, i * 128:(i + 1) * 128], rhs=Wt[:, c, :], start=(c == 0), stop=(c == NCH - 1)), r=[Wk, tk], w=[pk])
                    S.op("dve", lambda: V.tensor_tensor(out=h[:, i, nb * 512:(nb + 1) * 512], in0=h[:, i, nb * 512:(nb + 1) * 512], in1=ps[:, 0:512], op=ALU.add), r=[pk, hk[i]], w=[hk[i]])

        with Scope() as st:
            srcs = [(convT, c, convTk) for c in range(8)] + [(attnT, c, attnTk) for c in range(8)]
            proj_residual(st, w_out, srcs, "o")
            S.barrier()
        mem["ptr"] = mark_mix

        def dump(idx):
            if DBG:
                dk = S.key("dbg%d" % idx)
                for i in range(NT):
                    S.dma("sp", dbg[idx, i * 128:(i + 1) * 128, :], h[:, i, :], r=[hk[i]], w=[dk])

        dump(0)

        with Scope() as st:
            hcT = sb(st, "hcT", [128, NCH, NT * 128], BF16)
            hcTk = S.key("hcT")
            KcT = sb(st, "KcT", [128, NCH, 256], BF16)
            KcTk = S.key("KcT")
            Vc = sb(st, "Vc", [128, 2, D], BF16)
            Vck = S.key("Vc")
            Wb = [sb(st, "xWb%d" % i, [128, NCH, 512], BF16) for i in range(2)]
            Wbk = [S.key("xWb%d" % i) for i in range(2)]
            hn = [sb(st, "xhn%d" % i, [128, D], BF16) for i in range(2)]
            hnk = [S.key("xhn%d" % i) for i in range(2)]
            ss = [sb(st, "xss%d" % i, [128, 4], F32) for i in range(2)]
            ssk = [S.key("xss%d" % i) for i in range(2)]
            memscope = Scope()
            memscope.__enter__()
            xs = [sb(st, "xxs%d" % i, [128, D], F32) for i in range(2)]
            xsk = [S.key("xxs%d" % i) for i in range(2)]
            gme = sb(st, "gme", [128, D], F32)
            gmek = S.key("gme")
            mnT = sb(st, "mnT", [128, NCH, 256], BF16)
            mnTk = S.key("mnT")

            S.dma("sp", gme[:], bc(norm_mem_g, D), r=[], w=[gmek])
            for t in range(2):
                S.dma("sp", xs[t][:], memb[t * 128:(t + 1) * 128, :], r=[], w=[xsk[t]])
                rms_to_T(st, xs[t][:], xsk[t], gme, gmek, hn[t], hnk[t], hn[t], ss[t], ssk[t], mnT, mnTk, t * 128, evac_alt=t)
            wcount = 0
            for nb in range(4):
                Wt, Wk = Wb[wcount % 2], Wbk[wcount % 2]
                wcount += 1
                wblock(Wt, Wk, w_ckv, nb * 512)
                for j in range(4):
                    ps, pk = proj_featT(Wt, Wk, j * 128, mnT, mnTk, 0, 256, None, None)
                    evac(KcT[:, nb * 4 + j, :], ps[:, 0:256], r=[pk], w=[KcTk])
            for nb in range(4):
                Wt, Wk = Wb[wcount % 2], Wbk[wcount % 2]
                wcount += 1
                wblock(Wt, Wk, w_ckv, 2048 + nb * 512)
                for mt in range(2):
                    ps, pk = proj_tok(Wt, Wk, 512, mnT, mnTk, mt * 128)
                    evac(Vc[:, mt, nb * 512:(nb + 1) * 512], ps[:, 0:512], r=[pk], w=[Vck])
            S.barrier()
            memscope.__exit__(None, None, None)
            gcr = sb(st, "gcr", [128, D], F32)
            gcrk = S.key("gcr")
            QcT = sb(st, "QcT", [128, NCH, NT * 128], BF16)
            QcTk = S.key("QcT")
            PcT = [sb(st, "PcT%d" % i, [128, 512], BF16) for i in range(2)]
            PcTk = [S.key("PcT%d" % i) for i in range(2)]
            PcN = [sb(st, "PcN%d" % i, [128, 512], BF16) for i in range(2)]
            PcNk = [S.key("PcN%d" % i) for i in range(2)]
            rZ = sb(st, "rZ", [128, 512], F32)
            rZk = S.key("rZ")
            S.dma("sp", gcr[:], bc(norm_cross_g, D), r=[], w=[gcrk])
            rms_loop(NT, lambda i: (h[:, i, :], hk[i]), gcr, gcrk, hn, hnk, ss, ssk, hcT, hcTk)
            for nb in range(4):
                Wt, Wk = Wb[wcount % 2], Wbk[wcount % 2]
                wcount += 1
                wblock(Wt, Wk, w_cq, nb * 512)
                for j in range(4):
                    for half in range(2):
                        ps, pk = proj_featT(Wt, Wk, j * 128, hcT, hcTk, half * 512, 512, None, None)
                        evac(QcT[:, nb * 4 + j, half * 512:(half + 1) * 512], ps[:, 0:512], r=[pk], w=[QcTk])
            ocT, ocTk = hcT, hcTk
            sc = 512.0 ** -0.5
            for hd in range(4):
                for half in range(2):
                    for mt in range(2):
                        ps, pk = next_ps()
                        for cc in range(4):
                            S.op("pe", lambda cc=cc: PE.matmul(ps[:, 0:512], lhsT=KcT[:, 4 * hd + cc, mt * 128:(mt + 1) * 128], rhs=QcT[:, 4 * hd + cc, half * 512:(half + 1) * 512], start=(cc == 0), stop=(cc == 3)), r=[KcTk, QcTk], w=[pk])
                        S.op("act", lambda: A.activation(out=PcT[mt][:], in_=ps[:, 0:512], func=AF.Exp, scale=sc), r=[pk], w=[PcTk[mt]])
                    pz, pzk = next_ps()
                    for mt in range(2):
                        S.op("pe", lambda mt=mt: PE.matmul(pz[:, 0:512], lhsT=onesb[:], rhs=PcT[mt][:], start=(mt == 0), stop=(mt == 1)), r=[PcTk[mt], kc], w=[pzk])
                    S.op("dve", lambda: V.reciprocal(out=rZ[:], in_=pz[:, 0:512]), r=[pzk], w=[rZk])
                    for mt in range(2):
                        S.op("dve", lambda mt=mt: V.tensor_tensor(out=PcN[mt][:], in0=PcT[mt][:], in1=rZ[:], op=ALU.mult), r=[PcTk[mt], rZk], w=[PcNk[mt]])
                    for cc in range(4):
                        po, pok = next_ps()
                        for mt in range(2):
                            S.op("pe", lambda mt=mt, cc=cc: PE.matmul(po[:, 0:512], lhsT=Vc[:, mt, hd * 512 + cc * 128:hd * 512 + (cc + 1) * 128], rhs=PcN[mt][:], start=(mt == 0), stop=(mt == 1)), r=[Vck, PcNk[mt]], w=[pok])
                        evac(ocT[:, 4 * hd + cc, half * 512:(half + 1) * 512], po[:, 0:512], r=[pok], w=[ocTk])
            S.barrier()
            proj_residual(st, w_co, [(ocT, c, ocTk) for c in range(NCH)], "c", Wb, Wbk)
            S.barrier()
        dump(1)

        with Scope() as st:
            with Scope() as st1:
                v12a = sb(st1, "v12a", [128, NT, 2, 8, 16], F32)
                v12ak = S.key("v12a")
                scva = sb(st1, "scva", [128, NT, 8, 16], F32)
                scvak = S.key("scva")
                rZa = sb(st1, "rZa", [128, NT, 8], F32)
                rZak = S.key("rZa")
                sdk3 = [S.key("sdram%d" % i) for i in range(3)]

                def ps6():
                    i6 = ps_rr[0] % 6
                    ps_rr[0] += 1
                    return PS[i6], PSK[i6]

                with Scope() as st0:
                    hpT = sb(st0, "hpT", [128, NCH, NT * 128], BF16)
                    hpTk = S.key("hpT")
                    KpT = [sb(st0, "KpT%d" % i, [128, 8, 128], F32) for i in range(2)]
                    KpTk = [S.key("KpT%d" % i) for i in range(2)]
                    with Scope() as st00:
                        hn = [sb(st00, "phn%d" % i, [128, D], BF16) for i in range(2)]
                        hnk = [S.key("phn%d" % i) for i in range(2)]
                        gpe = sb(st00, "gpe", [128, D], F32)
                        gpek = S.key("gpe")
                        ss = [sb(st00, "pss%d" % i, [128, 4], F32) for i in range(2)]
                        ssk = [S.key("pss%d" % i) for i in range(2)]
                        knat = sb(st00, "knat", [128, 8, 128], F32)
                        knk = S.key("knat")
                        S.dma("sp", gpe[:], bc(norm_peer_g, D), r=[], w=[gpek])
                        rms_loop(NT, lambda i: (h[:, i, :], hk[i]), gpe, gpek, hn, hnk, ss, ssk, hpT, hpTk)
                        for si, kd in enumerate((keys1, keys2)):
                            S.dma("sp", knat[:], kd.rearrange("h n d -> n h d"), r=[], w=[knk])
                            for hh_ in range(8):
                                ps, pk = next_ps()
                                S.op("pe", lambda: PE.transpose(out=ps[:, 0:128], in_=knat[:, hh_, :], identity=identf[:]), r=[knk, kc], w=[pk])
                                evac(KpT[si][:, hh_, :], ps[:, 0:128], r=[pk], w=[KpTk[si]])
                        S.barrier()
                    Wb = [sb(st0, "pWb%d" % i, [128, NCH, 256], BF16) for i in range(2)]
                    Wbk = [S.key("pWb%d" % i) for i in range(2)]
                    Qh = [[sb(st0, "Qh%d_%d" % (p, j), [128, NT * 128], F32) for j in range(2)] for p in range(2)]
                    Qhk = [[S.key("Qh%d_%d" % (p, j)) for j in range(2)] for p in range(2)]
                    sst = [sb(st0, "sst%d" % i, [128, 2, 128], F32) for i in range(3)]
                    sstk = [S.key("sst%d" % i) for i in range(3)]
                    swk = [sb(st0, "swk%d" % i, [128, 128], F32) for i in range(2)]
                    swkk = [S.key("swk%d" % i) for i in range(2)]
                    cand = [sb(st0, "cand%d" % i, [128, 256], F32) for i in range(2)]
                    candk = [S.key("cand%d" % i) for i in range(2)]
                    esc = sb(st0, "esc", [128, NT * 8, 16], F32)
                    esck = S.key("esc")
                    zsum = sb(st0, "zsum", [128, NT * 8], F32)
                    un = 0
                    for nb in range(8):
                        Wt, Wk = Wb[nb % 2], Wbk[nb % 2]
                        wblock(Wt, Wk, w_pq, nb * 256, ncols=256)
                        for j in range(2):
                            for half in range(2):
                                ps, pk = ps6()
                                for c in range(NCH):
                                    S.op("pe", lambda c=c: PE.matmul(ps[:, 0:512], lhsT=Wt[:, c, j * 128:(j + 1) * 128], rhs=hpT[:, c, half * 512:(half + 1) * 512], start=(c == 0), stop=(c == NCH - 1)), r=[Wk, hpTk], w=[pk])
                                S.op("act", lambda: A.copy(out=Qh[nb % 2][j][:, half * 512:(half + 1) * 512], in_=ps[:, 0:512]), r=[pk], w=[Qhk[nb % 2][j]])
                        for i in range(NT):
                            st_, stk_ = sst[un % 3], sstk[un % 3]
                            sw_, swk_ = swk[un % 2], swkk[un % 2]
                            cd_, cdk_ = cand[un % 2], candk[un % 2]
                            un += 1
                            ps, pk = ps6()
                            for side in range(2):
                                S.op("pe", lambda: PE.matmul(ps[:, side * 128:(side + 1) * 128], lhsT=Qh[nb % 2][side][:, i * 128:(i + 1) * 128], rhs=KpT[side][:, nb, :], start=True, stop=True), r=[Qhk[nb % 2][side], KpTk[side]], w=[pk])
                            S.op("act", lambda: A.copy(out=st_[:], in_=ps[:, 0:256].rearrange("p (s n) -> p s n", s=2)), r=[pk], w=[stk_])
                            S.dma("sp", sdram[i, :, :, nb, :], st_[:], r=[stk_], w=[sdk3[(un - 1) % 3]])
                            for side in range(2):
                                vX = v12a[:, i, side, nb, :]
                                S.op("dve", lambda: V.max(out=vX[:, 0:8], in_=st_[:, side, :]), r=[stk_], w=[v12ak])
                                S.op("dve", lambda: V.match_replace(out=sw_[:], in_to_replace=vX[:, 0:8], in_values=st_[:, side, :], imm_value=-1e30), r=[stk_, v12ak], w=[swk_])
                                S.op("dve", lambda: V.max(out=vX[:, 8:16], in_=sw_[:]), r=[swk_], w=[v12ak])
                            S.op("pool", lambda: G.tensor_tensor(out=cd_[:].rearrange("p (i j) -> p i j", i=16), in0=v12a[:, i, 0, nb, :].unsqueeze(2).to_broadcast([128, 16, 16]), in1=v12a[:, i, 1, nb, :].unsqueeze(1).to_broadcast([128, 16, 16]), op=ALU.add), r=[v12ak], w=[cdk_])
                            sX = scva[:, i, nb, :]
                            S.op("dve", lambda: V.max(out=sX[:, 0:8], in_=cd_[:]), r=[cdk_], w=[scvak])
                            S.op("dve", lambda: V.match_replace(out=cd_[:], in_to_replace=sX[:, 0:8], in_values=cd_[:], imm_value=-1e30), r=[cdk_, scvak], w=[cdk_])
                            S.op("dve", lambda: V.max(out=sX[:, 8:16], in_=cd_[:]), r=[cdk_], w=[scvak])
                    scf = scva[:].rearrange("p i h r -> p (i h) r")
                    S.op("dve", lambda: V.tensor_tensor(out=esc[:], in0=scf, in1=scf[:, :, 0:1].to_broadcast([128, NT * 8, 16]), op=ALU.subtract), r=[scvak], w=[esck])
                    S.op("act", lambda: A.activation(out=esc[:], in_=esc[:], func=AF.Exp), r=[esck], w=[esck])
                    S.op("dve", lambda: V.reduce_sum(out=zsum[:], in_=esc[:], axis=AX.X), r=[esck], w=[esck])
                    S.op("dve", lambda: V.reciprocal(out=rZa[:].rearrange("p i h -> p (i h)"), in_=zsum[:]), r=[esck], w=[rZak])
                    S.barrier()

                s12 = [sb(st1, "s12_%d" % i, [128, 2, 8, 128], F32) for i in range(2)]
                s12k = [S.key("s12_%d" % i) for i in range(2)]
                sumQ2 = [sb(st1, "sumQ%d" % i, [128, 8, 16, 16], F32) for i in range(2)]
                sumQ2k = [S.key("sumQ%d" % i) for i in range(2)]
                tmb = [sb(st1, "tmb%d" % i, [128, 8, 16, 16], BF16) for i in range(3)]
                tmbk = [S.key("tmb%d" % i) for i in range(3)]
                rot = {"q": 0, "t": 0, "b": 0}
                Qk_ = sb(st1, "Qk", [128, 128, 128], BF16)
                Qkk = S.key("Qk")
                P1k = sb(st1, "P1k", [128, 128, 128], BF16)
                P1kk = S.key("P1k")
                Wall = sb(st1, "Wall", [128, 64, 128], BF16)
                Wallk = S.key("Wall")
                E2 = sb(st1, "E2", [128, 8, 128], BF16)
                E2k = S.key("E2")
                Af = sb(st1, "Af", [128, 8, 16], F32)
                Afk = S.key("Af")
                AT = sb(st1, "AT", [128, 128], F32)
                ATk = S.key("AT")
                wdk = S.key("wd")
                if DBG_MEM:
                    print("phase2 mem ptr", mem["ptr"], "limit", mem["limit"], "free", mem["limit"] - mem["ptr"])
                S.dma("sp", s12[0][:], sdram[0, :, :, :, :], r=sdk3, w=[s12k[0]])
                for tile_i in range(NT):
                    if tile_i + 1 < NT:
                        S.dma("sp", s12[(tile_i + 1) % 2][:], sdram[tile_i + 1, :, :, :, :], r=sdk3, w=[s12k[(tile_i + 1) % 2]])
                    sk_ = s12k[tile_i % 2]
                    s1, s2 = s12[tile_i % 2][:, 0, :, :], s12[tile_i % 2][:, 1, :, :]
                    v1, v2 = v12a[:, tile_i, 0, :, :], v12a[:, tile_i, 1, :, :]
                    S.op("dve", lambda: V.tensor_tensor(out=Af[:], in0=v1, in1=v1[:, :, 0:1].to_broadcast([128, 8, 16]), op=ALU.subtract), r=[v12ak], w=[Afk])
                    S.op("act", lambda: A.activation(out=Af[:], in_=Af[:], func=AF.Exp), r=[Afk], w=[Afk])
                    S.op("dve", lambda: V.tensor_tensor(out=Af[:], in0=Af[:], in1=rZa[:, tile_i, :].unsqueeze(2).to_broadcast([128, 8, 16]), op=ALU.mult), r=[Afk, rZak], w=[Afk])
                    ps, pk = ps6()
                    S.op("pe", lambda: PE.transpose(out=ps[:, 0:128], in_=Af[:].rearrange("p h i -> p (h i)"), identity=identf[:]), r=[Afk, kc], w=[pk])
                    S.op("act", lambda: A.copy(out=AT[:], in_=ps[:, 0:128]), r=[pk], w=[ATk])
                    sq0, sq0k = sumQ2[rot["q"] % 2], sumQ2k[rot["q"] % 2]
                    rot["q"] += 1
                    tmpE = sq0[:].rearrange("p h i b -> p (h i b)")[:, 0:1024].rearrange("p (h b) -> p h b", h=8)
                    S.op("dve", lambda: V.tensor_tensor(out=tmpE, in0=s2, in1=v2[:, :, 0:1].to_broadcast([128, 8, 128]), op=ALU.subtract), r=[sk_, v12ak], w=[sq0k])
                    S.op("act", lambda: A.activation(out=E2[:], in_=tmpE, func=AF.Exp), r=[sq0k], w=[E2k])
                    for r8 in range(8):
                        sq, sqk = sumQ2[rot["q"] % 2], sumQ2k[rot["q"] % 2]
                        rot["q"] += 1
                        tb, tbk = tmb[rot["t"] % 3], tmbk[rot["t"] % 3]
                        rot["t"] += 1
                        S.op("pool", lambda: G.tensor_tensor(out=sq[:], in0=v1.unsqueeze(3).to_broadcast([128, 8, 16, 16]), in1=s2[:, :, r8 * 16:(r8 + 1) * 16].unsqueeze(2).to_broadcast([128, 8, 16, 16]), op=ALU.add), r=[v12ak, sk_], w=[sqk])
                        for hh_ in range(8):
                            S.op("dve", lambda: V.scalar_tensor_tensor(out=tb[:, hh_, :, :], in0=sq[:, hh_, :, :], scalar=scva[:, tile_i, hh_, 15:16], in1=E2[:, hh_, r8 * 16:(r8 + 1) * 16].unsqueeze(1).to_broadcast([128, 16, 16]), op0=ALU.is_ge, op1=ALU.mult), r=[sqk, scvak, E2k], w=[tbk])
                        Qv = tb[:].rearrange("p h i b -> p (h i) b")
                        for b8 in range(2):
                            bank, bankk = TP[rot["b"] % 2], TPK[rot["b"] % 2]
                            rot["b"] += 1
                            for bb in range(8):
                                S.op("pe", lambda: PE.transpose(out=bank[:, bb * 128:(bb + 1) * 128], in_=Qv[:, :, b8 * 8 + bb], identity=ident[:]), r=[tbk, kc], w=[bankk])
                            b0 = r8 * 16 + b8 * 8
                            S.op("act", lambda: A.copy(out=Qk_[:, :, b0:b0 + 8], in_=bank[:, :].rearrange("p (b t) -> p t b", b=8)), r=[bankk], w=[Qkk])
                        tb, tbk = tmb[rot["t"] % 3], tmbk[rot["t"] % 3]
                        rot["t"] += 1
                        S.op("dve", lambda: V.tensor_tensor(out=tb[:], in0=s1[:, :, r8 * 16:(r8 + 1) * 16].unsqueeze(2).to_broadcast([128, 8, 16, 16]), in1=v1.unsqueeze(3).to_broadcast([128, 8, 16, 16]), op=ALU.is_equal), r=[sk_, v12ak], w=[tbk])
                        Pv = tb[:].rearrange("p h i a -> p (h i) a")
                        for a8 in range(2):
                            bank, bankk = TP[rot["b"] % 2], TPK[rot["b"] % 2]
                            rot["b"] += 1
                            for aa in range(8):
                                S.op("pe", lambda: PE.transpose(out=bank[:, aa * 128:(aa + 1) * 128], in_=Pv[:, :, a8 * 8 + aa], identity=ident[:]), r=[tbk, kc], w=[bankk])
                            a0_ = r8 * 16 + a8 * 8
                            S.op("dve", lambda: V.tensor_tensor(out=P1k[:, :, a0_:a0_ + 8], in0=bank[:, :].rearrange("p (a t) -> p t a", a=8), in1=AT[:].unsqueeze(2).to_broadcast([128, 128, 8]), op=ALU.mult), r=[bankk, ATk], w=[P1kk])
                    for ah in range(2):
                        for t8 in range(16):
                            ps, pk = ps6()
                            for tt in range(8):
                                t_ = t8 * 8 + tt
                                S.op("pe", lambda: PE.matmul(ps[:, tt * 64:(tt + 1) * 64], lhsT=Qk_[:, t_, :], rhs=P1k[:, t_, ah * 64:(ah + 1) * 64], start=True, stop=True), r=[Qkk, P1kk], w=[pk])
                            S.op("act", lambda: A.copy(out=Wall[:, :, t8 * 8:(t8 + 1) * 8], in_=ps[:, 0:512].rearrange("p (t a) -> p a t", t=8)), r=[pk], w=[Wallk])
                        for a4 in range(4):
                            a0 = ah * 64 + a4 * 16
                            S.dma("sp", wd[a0:a0 + 16, :, tile_i * 128:(tile_i + 1) * 128].rearrange("a b t -> b a t"), Wall[:, a4 * 16:(a4 + 1) * 16, :], r=[Wallk], w=[wdk])
                S.barrier()

            with Scope() as st2:
                GRP = 4
                NSL = 3
                Uc = [sb(st2, "Uc%d" % i, [128, D], BF16) for i in range(NSL)]
                Uck = [S.key("Uc%d" % i) for i in range(NSL)]
                UT = [sb(st2, "UT%d" % i, [128, NCH, 128], BF16) for i in range(2)]
                UTk = [S.key("UT%d" % i) for i in range(2)]
                Vg = [[sb(st2, "Vg%d_%d" % (p, i), [128, D], BF16) for i in range(GRP)] for p in range(2)]
                Vgk = [[S.key("Vg%d_%d" % (p, i)) for i in range(GRP)] for p in range(2)]
                Wc = [sb(st2, "Wc%d" % i, [128, NT * 128], BF16) for i in range(NSL)]
                Wck = [S.key("Wc%d" % i) for i in range(NSL)]
                gl = [sb(st2, "gl%d" % i, [128, NT * 128], BF16) for i in range(2)]
                glk = [S.key("gl%d" % i) for i in range(2)]
                GH = [sb(st2, "GH%d" % i, [128, NT * 128], BF16) for i in range(2 * GRP)]
                GHk = [S.key("GH%d" % i) for i in range(2 * GRP)]
                Hb = (PS[4], PS[5])
                Hbk = (PSK[4], PSK[5])
                hpT = sb(st2, "hpT2", [128, NCH, NT * 128], BF16)
                hpTk = S.key("hpT2")
                with Scope() as st3:
                    hn = [sb(st3, "qhn%d" % i, [128, D], BF16) for i in range(2)]
                    hnk = [S.key("qhn%d" % i) for i in range(2)]
                    gpe = sb(st3, "qgpe", [128, D], F32)
                    gpek = S.key("qgpe")
                    ss = [sb(st3, "qss%d" % i, [128, 4], F32) for i in range(2)]
                    ssk = [S.key("qss%d" % i) for i in range(2)]
                    S.dma("sp", gpe[:], bc(norm_peer_g, D), r=[], w=[gpek])
                    rms_loop(NT, lambda i: (h[:, i, :], hk[i]), gpe, gpek, hn, hnk, ss, ssk, hpT, hpTk)
                    S.barrier()

                def prefetch(ch):
                    s = ch % NSL
                    S.dma("pool", Uc[s][:], peer_u[ch * 128:(ch + 1) * 128, :], r=[], w=[Uck[s]])
                    p, i_ = (ch // GRP) % 2, ch % GRP
                    S.dma("pool", Vg[p][i_][:], peer_v[ch * 128:(ch + 1) * 128, :], r=[], w=[Vgk[p][i_]])
                    S.dma("sp", Wc[s][:], wd[ch, :, :], r=[wdk], w=[Wck[s]])

                prefetch(0)
                prefetch(1)
                NCHK = 128

                def do_T(ch):
                    s_ = ch % NSL
                    ut, utk = UT[ch % 2], UTk[ch % 2]
                    for half in range(2):
                        for c in range(8):
                            S.op("pe", lambda c=c: PE.transpose(out=TP[half][:, c * 128:(c + 1) * 128], in_=Uc[s_][:, (half * 8 + c) * 128:(half * 8 + c + 1) * 128], identity=ident[:]), r=[Uck[s_], kc], w=[TPK[half]])
                        evac(ut[:, half * 8:(half + 1) * 8, :], TP[half][:, :].rearrange("p (c n) -> p c n", c=8), r=[TPK[half]], w=[utk])

                def do_H(ch):
                    s_ = ch % NSL
                    ut, utk = UT[ch % 2], UTk[ch % 2]
                    gh, ghk = GH[ch % (2 * GRP)], GHk[ch % (2 * GRP)]
                    for half in range(2):
                        for c in range(NCH):
                            S.op("pe", lambda c=c: PE.matmul(Hb[half][:, 0:512], lhsT=ut[:, c, :], rhs=hpT[:, c, half * 512:(half + 1) * 512], start=(c == 0), stop=(c == NCH - 1)), r=[utk, hpTk], w=[Hbk[half]])
                        S.op("act", lambda: A.activation(out=gl[ch % 2][:, half * 512:(half + 1) * 512], in_=Hb[half][:, 0:512], func=AF.Gelu), r=[Hbk[half]], w=[glk[ch % 2]])
                    S.op("dve", lambda: V.tensor_tensor(out=gh[:], in0=gl[ch % 2][:], in1=Wc[s_][:], op=ALU.mult), r=[glk[ch % 2], Wck[s_]], w=[ghk])

                def do_Y(gi):
                    p = gi % 2
                    for i in range(NT):
                        for nb in range(4):
                            for ci in range(GRP):
                                gh, ghk = GH[(gi * GRP + ci) % (2 * GRP)], GHk[(gi * GRP + ci) % (2 * GRP)]
                                S.op("pe", lambda ci=ci, nb=nb: PE.matmul(PS[nb][:, 0:512], lhsT=gh[:, i * 128:(i + 1) * 128], rhs=Vg[p][ci][:, nb * 512:(nb + 1) * 512], start=(ci == 0), stop=(ci == GRP - 1)), r=[ghk, Vgk[p][ci]], w=[PSK[nb]])
                            S.op("dve", lambda nb=nb: V.tensor_tensor(out=h[:, i, nb * 512:(nb + 1) * 512], in0=h[:, i, nb * 512:(nb + 1) * 512], in1=PS[nb][:, 0:512], op=ALU.add), r=[PSK[nb], hk[i]], w=[hk[i]])

                do_T(0)
                for ch in range(NCHK):
                    if ch + 2 < NCHK:
                        prefetch(ch + 2)
                    if ch + 1 < NCHK:
                        do_T(ch + 1)
                    do_H(ch)
                    if ch % GRP == 0 and ch >= GRP:
                        do_Y(ch // GRP - 1)
                do_Y(NCHK // GRP - 1)
                S.barrier()
        dump(2)

        with Scope() as st:
            gf = sb(st, "gf", [128, D], F32)
            gfk = S.key("gf")
            yo = [sb(st, "yo%d" % i, [128, D], F32) for i in range(2)]
            yok = [S.key("yo%d" % i) for i in range(2)]
            sqj = sb(st, "fsq", [128, D], BF16)
            sqk = S.key("fsq")
            ss = [sb(st, "fss%d" % i, [128, 4], F32) for i in range(2)]
            ssk = [S.key("fss%d" % i) for i in range(2)]
            yk2 = [S.key("y%d" % i) for i in range(2)]
            S.dma("sp", gf[:], bc(final_g, D), r=[], w=[gfk])
            for i in range(NT):
                u = i % 2
                S.op("act", lambda: A.activation(out=sqj[:], in_=h[:, i, :], func=AF.Square, accum_out=ss[u][:, 0:1]), r=[hk[i]], w=[sqk, ssk[u]])
                S.op("dve", lambda: V.tensor_scalar(out=ss[u][:, 1:2], in0=ss[u][:, 0:1], scalar1=1.0 / D, scalar2=RMS_EPS, op0=ALU.mult, op1=ALU.add), r=[ssk[u]], w=[ssk[u]])
                S.op("act", lambda: A.activation(out=ss[u][:, 2:3], in_=ss[u][:, 1:2], func=AF.Sqrt), r=[ssk[u]], w=[ssk[u]])
                S.op("dve", lambda: V.reciprocal(out=ss[u][:, 3:4], in_=ss[u][:, 2:3]), r=[ssk[u]], w=[ssk[u]])
                S.op("dve", lambda: V.scalar_tensor_tensor(out=yo[u][:], in0=h[:, i, :], scalar=ss[u][:, 3:4], in1=gf[:], op0=ALU.mult, op1=ALU.mult), r=[hk[i], ssk[u], gfk], w=[yok[u]])
                S.dma("sp", y_out[i * 128:(i + 1) * 128, :], yo[u][:], r=[yok[u]], w=[yk2[u]])
            S.barrier()
    return nc


_NC_CACHE = {}


def kernel(**inputs):
    x = np.ascontiguousarray(np.asarray(inputs["x"], dtype=np.float32))
    mem = np.ascontiguousarray(np.asarray(inputs["mem"], dtype=np.float32))
    B, Sq, Dm = x.shape
    shared = {}
    for name in ("norm_mix_g", "w_in", "conv_dw_w", "conv_dw_b", "conv_ln_g", "conv_ln_b", "lambda_q1", "lambda_k1",
                 "lambda_q2", "lambda_k2", "diff_subln_g", "w_out", "norm_cross_g", "norm_mem_g", "w_cq", "w_ckv",
                 "w_co", "norm_peer_g", "w_pq", "peer_keys1", "peer_keys2", "peer_u", "peer_v"):
        a = np.asarray(inputs[name], dtype=np.float32)
        shared[name] = np.ascontiguousarray(a[0])
    shared["final_norm_g"] = np.ascontiguousarray(np.asarray(inputs["final_norm_g"], dtype=np.float32))
    in_maps = []
    for c in range(8):
        b, j = c // 4, c % 4
        start = j * 1024
        xwin = np.zeros((NW * 128, Dm), np.float32)
        val = np.zeros((128, NW), np.float32)
        lo = start - 24 * 128
        for w in range(NW):
            p0 = lo + w * 128
            if p0 >= 0:
                xwin[w * 128:(w + 1) * 128] = x[b, p0:p0 + 128]
                val[:, w] = 1.0
        m = dict(shared)
        m["xw"] = xwin
        m["valid"] = val
        m["memb"] = mem[b]
        in_maps.append(m)
    if "nc" not in _NC_CACHE:
        _NC_CACHE["nc"] = build()
    nc = _NC_CACHE["nc"]
    res = run_bass_kernel_spmd(nc, in_maps, core_ids=list(range(8)))
    out = np.zeros((B, Sq, Dm), np.float32)
    for c in range(8):
        b, j = c // 4, c % 4
        out[b, j * 1024:(j + 1) * 1024] = res.results[c]["y"]
    if DBG:
        kernel.dbg = [res.results[c]["dbg"] for c in range(8)]
    return out
```

```python
import numpy as np
from contextlib import ExitStack
import concourse.bass as bass
import concourse.mybir as mybir
from concourse.bass_utils import run_bass_kernel_spmd

F32, BF16 = mybir.dt.float32, mybir.dt.bfloat16
AF = mybir.ActivationFunctionType
ALU = mybir.AluOpType
AX = mybir.AxisListType

D = 2048
NT = 8
NW = 32
NCH = 16
RMS_EPS = 1e-6
LN_EPS = 1e-5
LAM_INIT = 0.8 - 0.6 * 1.0
DBG = False
DBG_MEM = False


class Key:
    __slots__ = ("name", "w", "r", "sem", "tot")

    def __init__(self, name):
        self.name = name
        self.w = None
        self.r = {}
        self.sem = None
        self.tot = 0


class Sched:
    ENG = ("pe", "act", "dve", "pool", "sp")

    def __init__(self, nc, es):
        self.nc = nc
        self.es = es
        self.eng = {"pe": nc.tensor, "act": nc.scalar, "dve": nc.vector, "pool": nc.gpsimd, "sp": nc.sync}
        self.semobj = {}
        for e in self.ENG:
            self.semobj[e] = es.enter_context(nc.semaphore("s_" + e))
        self.cnt = {e: 0 for e in self.ENG}
        self.waited = {e: {} for e in self.ENG}
        self.clock = {}
        self.dkeys = []

    def key(self, name):
        return Key(name)

    def _need(self, eng, ev):
        sk, val = ev
        w = self.waited[eng]
        if w.get(sk, 0) >= val:
            return
        self.eng[eng].wait_ge(self.semobj[sk], val)
        w[sk] = val
        snap = self.clock.get(ev)
        if snap:
            for k2, v2 in snap.items():
                if w.get(k2, 0) < v2:
                    w[k2] = v2

    def _deps(self, eng, r, w, dma=False):
        for k in r:
            if k.w is not None and not (eng == "pe" and k.w[0] == "pe"):
                self._need(eng, k.w)
        for k in w:
            if k.w is not None:
                if eng == "pe" and k.w[0] == "pe":
                    pass
                elif dma and k.sem is not None and k.w[0] == k.sem:
                    pass
                else:
                    self._need(eng, k.w)
            for sk, val in k.r.items():
                if sk == eng and eng == "pe":
                    continue
                self._need(eng, (sk, val))

    def op(self, eng, fn, r=(), w=()):
        self._deps(eng, r, w)
        inst = fn()
        inst.then_inc(self.semobj[eng], 1)
        self.cnt[eng] += 1
        ev = (eng, self.cnt[eng])
        snap = dict(self.waited[eng])
        snap[eng] = self.cnt[eng]
        self.clock[ev] = snap
        for k in r:
            if k.r.get(eng, 0) < ev[1]:
                k.r[eng] = ev[1]
        for k in w:
            k.w = ev
            k.r = {}
        return inst

    def dma(self, q, out, in_, r, w):
        (wk,) = w
        if wk.sem is None:
            name = "d%d" % len(self.dkeys)
            self.semobj[name] = self.es.enter_context(self.nc.semaphore("s_" + name))
            wk.sem = name
            self.dkeys.append(wk)
        self._deps(q, r, w, dma=True)
        inst = self.eng[q].dma_start(out=out, in_=in_)
        inst.then_inc(self.semobj[wk.sem], 16)
        wk.tot += 16
        ev = (wk.sem, wk.tot)
        self.clock[ev] = dict(self.waited[q])
        for k in r:
            k.r[wk.sem] = wk.tot
        wk.w = ev
        wk.r = {}
        return inst

    def barrier(self):
        for e in self.ENG:
            for e2 in ("pe", "act", "dve", "pool"):
                if self.cnt[e2] > 0:
                    self._need(e, (e2, self.cnt[e2]))
            for k in self.dkeys:
                if k.tot > 0:
                    self._need(e, (k.sem, k.tot))


def build():
    nc = bass.Bass("TRN2", target_bir_lowering=False)

    def din(name, shape):
        return nc.dram_tensor(name, shape, F32, kind="ExternalInput").ap()

    xw = din("xw", [NW * 128, D])
    valid = din("valid", [128, NW])
    memb = din("memb", [256, D])
    norm_mix_g = din("norm_mix_g", [D])
    w_in = din("w_in", [D, 5120])
    conv_dw_w = din("conv_dw_w", [31, 1024])
    conv_dw_b = din("conv_dw_b", [1024])
    conv_ln_g = din("conv_ln_g", [1024])
    conv_ln_b = din("conv_ln_b", [1024])
    lq1 = din("lambda_q1", [64])
    lk1 = din("lambda_k1", [64])
    lq2 = din("lambda_q2", [64])
    lk2 = din("lambda_k2", [64])
    subln = din("diff_subln_g", [128])
    w_out = din("w_out", [D, D])
    norm_cross_g = din("norm_cross_g", [D])
    norm_mem_g = din("norm_mem_g", [D])
    w_cq = din("w_cq", [D, D])
    w_ckv = din("w_ckv", [D, 2 * D])
    w_co = din("w_co", [D, D])
    norm_peer_g = din("norm_peer_g", [D])
    w_pq = din("w_pq", [D, D])
    keys1 = din("peer_keys1", [8, 128, 128])
    keys2 = din("peer_keys2", [8, 128, 128])
    peer_u = din("peer_u", [16384, D])
    peer_v = din("peer_v", [16384, D])
    final_g = din("final_norm_g", [D])
    y_out = nc.dram_tensor("y", [NT * 128, D], F32, kind="ExternalOutput").ap()
    wd = nc.dram_tensor("wd_scratch", [128, 128, NT * 128], BF16, kind="Internal").ap()
    sdram = nc.dram_tensor("s_scratch", [NT, 128, 2, 8, 128], F32, kind="Internal").ap()
    dbg = None
    if DBG:
        dbg = nc.dram_tensor("dbg", [3, NT * 128, D], F32, kind="ExternalOutput").ap()

    with ExitStack() as es:
        S = Sched(nc, es)
        V, A, G, PE = nc.vector, nc.scalar, nc.gpsimd, nc.tensor

        SB_BASE, SB_TOP = 16512, 229344
        mem = {"ptr": SB_BASE, "n": 0, "limit": SB_TOP}

        class Scope:
            def __enter__(self):
                self.mark = mem["ptr"]
                return self

            def __exit__(self, *a):
                mem["ptr"] = self.mark
                return False

        def sb(stack, name, shape, dtype, limit=None):
            nbytes = int(np.prod(shape[1:])) * (4 if dtype == F32 else 2)
            nbytes = (nbytes + 63) // 64 * 64
            off = mem["ptr"]
            lim = mem["limit"]
            assert off + nbytes <= lim, ("SBUF overflow", name, off, nbytes, lim)
            mem["ptr"] = off + nbytes
            mem["n"] += 1
            return nc.alloc_sbuf_tensor_at("%s_%d" % (name, mem["n"]), list(shape), dtype, offset=off)

        H_OFF = SB_TOP - NT * D * 4

        def bc(ap1d, n):
            return ap1d.rearrange("(o n) -> o n", o=1).to_broadcast([128, n])

        PS = [es.enter_context(nc.psum_tensor("ps%d" % i, [128, 512], F32)) for i in range(6)]
        PSK = [S.key("ps%d" % i) for i in range(6)]
        TP = [es.enter_context(nc.psum_tensor("tp%d" % i, [128, 1024], BF16)) for i in range(2)]
        TPK = [S.key("tp%d" % i) for i in range(2)]

        identf = sb(es, "identf", [128, 128], F32)
        ident = sb(es, "ident", [128, 128], BF16)
        tri = sb(es, "tri", [128, 128], BF16)
        zer = sb(es, "zer", [128, 512], BF16)
        onesb = sb(es, "onesb", [128, 128], BF16)
        onesf = sb(es, "onesf", [128, 128], F32)
        epsr = sb(es, "epsr", [128, 1], F32)
        epsl = sb(es, "epsl", [128, 1], F32)
        kc = S.key("consts")
        S.op("pool", lambda: G.memset(identf[:], 0.0), w=[kc])
        S.op("pool", lambda: G.affine_select(out=identf[:], in_=identf[:], pattern=[[-1, 128]], compare_op=ALU.not_equal,
                                             fill=1.0, base=0, channel_multiplier=1), r=[kc], w=[kc])
        S.op("dve", lambda: V.tensor_copy(out=ident[:], in_=identf[:]), r=[kc], w=[kc])
        S.op("pool", lambda: G.memset(onesf[:], 1.0), w=[kc])
        S.op("pool", lambda: G.affine_select(out=onesf[:], in_=onesf[:], pattern=[[1, 128]], compare_op=ALU.is_ge,
                                             fill=0.0, base=0, channel_multiplier=-1), r=[kc], w=[kc])
        S.op("dve", lambda: V.tensor_copy(out=tri[:], in_=onesf[:]), r=[kc], w=[kc])
        S.op("dve", lambda: V.memset(onesf[:], 1.0 / 128.0), r=[kc], w=[kc])
        S.op("dve", lambda: V.memset(zer[:], 0.0), w=[kc])
        S.op("dve", lambda: V.memset(onesb[:], 1.0), w=[kc])
        S.op("dve", lambda: V.memset(epsr[:], RMS_EPS), w=[kc])
        S.op("dve", lambda: V.memset(epsl[:], LN_EPS), w=[kc])

        sm_i = [0]

        def rms_A(src, srck, gbc, gk, hn, hnk, ss, ssk):
            S.op("act", lambda: A.activation(out=hn[:], in_=src, func=AF.Square, accum_out=ss[:, 0:1]), r=[srck], w=[hnk, ssk])
            S.op("dve", lambda: V.tensor_scalar(out=ss[:, 1:2], in0=ss[:, 0:1], scalar1=1.0 / D, scalar2=RMS_EPS, op0=ALU.mult, op1=ALU.add), r=[ssk], w=[ssk])
            S.op("act", lambda: A.activation(out=ss[:, 2:3], in_=ss[:, 1:2], func=AF.Sqrt), r=[ssk], w=[ssk])
            S.op("dve", lambda: V.reciprocal(out=ss[:, 3:4], in_=ss[:, 2:3]), r=[ssk], w=[ssk])
            S.op("dve", lambda: V.scalar_tensor_tensor(out=hn[:], in0=src, scalar=ss[:, 3:4], in1=gbc[:], op0=ALU.mult, op1=ALU.mult), r=[srck, ssk, gk], w=[hnk])

        def rms_B(hn, hnk, dstT, dstk, col0, evac_alt=0):
            for half in range(2):
                for c in range(8):
                    S.op("pe", lambda c=c: PE.transpose(out=TP[half][:, c * 128:(c + 1) * 128], in_=hn[:, (half * 8 + c) * 128:(half * 8 + c + 1) * 128], identity=ident[:]), r=[hnk, kc], w=[TPK[half]])
                dst = dstT[:, half * 8:(half + 1) * 8, col0:col0 + 128]
                srcp = TP[half][:, :].rearrange("p (c n) -> p c n", c=8)
                if (half + evac_alt) % 2 == 0:
                    S.op("act", lambda: A.copy(out=dst, in_=srcp), r=[TPK[half]], w=[dstk])
                else:
                    S.op("dve", lambda: V.tensor_copy(out=dst, in_=srcp), r=[TPK[half]], w=[dstk])

        def rms_to_T(st, src, srck, gbc, gk, hn, hnk, sqj, ss, ssk, dstT, dstk, col0, evac_alt=0):
            rms_A(src, srck, gbc, gk, hn, hnk, ss, ssk)
            rms_B(hn, hnk, dstT, dstk, col0, evac_alt)

        def rms_loop(n, src_fn, gbc, gk, hn2, hn2k, ss2, ss2k, dstT, dstk):
            a0, k0 = src_fn(0)
            rms_A(a0, k0, gbc, gk, hn2[0], hn2k[0], ss2[0], ss2k[0])
            for i in range(n):
                if i + 1 < n:
                    a1, k1 = src_fn(i + 1)
                    rms_A(a1, k1, gbc, gk, hn2[(i + 1) % 2], hn2k[(i + 1) % 2], ss2[(i + 1) % 2], ss2k[(i + 1) % 2])
                rms_B(hn2[i % 2], hn2k[i % 2], dstT, dstk, i * 128, evac_alt=i)

        def wblock(Wt, Wk, wdram, c0, ncols=512):
            S.dma("pool", Wt[:, :, 0:ncols], wdram[:, c0:c0 + ncols].rearrange("(c p) n -> p c n", p=128), r=[], w=[Wk])

        ps_rr = [0]

        def next_ps():
            i = ps_rr[0] % 6
            ps_rr[0] += 1
            return PS[i], PSK[i]

        evac_rr = [0]

        def evac(out, in_, r, w):
            evac_rr[0] += 1
            if evac_rr[0] % 2 == 0:
                S.op("act", lambda: A.copy(out=out, in_=in_), r=r, w=w)
            else:
                S.op("dve", lambda: V.tensor_copy(out=out, in_=in_), r=r, w=w)

        def proj_featT(Wt, Wk, wc0, actT, actk, t0, n, dst, dstk):
            ps, pk = next_ps()
            for c in range(NCH):
                S.op("pe", lambda c=c: PE.matmul(ps[:, 0:n], lhsT=Wt[:, c, wc0:wc0 + 128], rhs=actT[:, c, t0:t0 + n], start=(c == 0), stop=(c == NCH - 1)), r=[Wk, actk], w=[pk])
            return ps, pk

        def proj_tok(Wt, Wk, ncols, actT, actk, t0):
            ps, pk = next_ps()
            for c in range(NCH):
                S.op("pe", lambda c=c: PE.matmul(ps[:, 0:ncols], lhsT=actT[:, c, t0:t0 + 128], rhs=Wt[:, c, 0:ncols], start=(c == 0), stop=(c == NCH - 1)), r=[Wk, actk], w=[pk])
            return ps, pk

        mark_mix = None
        attnT = sb(es, "attnT", [128, 8, NT * 128], BF16)
        attnTk = S.key("attnT")
        mark_mix = mem["ptr"] - 8 * NT * 128 * 2

        with Scope() as st:
            KT = sb(st, "KT", [128, 4, NW * 128], BF16)
            VA = sb(st, "VA", [128, NW, 4, 128], BF16)
            QT = sb(st, "QT", [128, 4, NT * 128], BF16)
            KTk, VAk, QTk = S.key("KT"), S.key("VA"), S.key("QT")
            hnT = [sb(st, "hnT%d" % i, [128, NCH, 256], BF16) for i in range(2)]
            hnTk = [S.key("hnT%d" % i) for i in range(2)]
            Wb = [sb(st, "Wb%d" % i, [128, NCH, 512], BF16) for i in range(3)]
            Wbk = [S.key("Wb%d" % i) for i in range(3)]
            xs_off = mem["ptr"]
            xs = [sb(st, "xs%d" % i, [128, D], F32) for i in range(2)]
            xsk = [S.key("xs%d" % i) for i in range(2)]
            hn2 = [sb(st, "hn%d" % i, [128, D], BF16) for i in range(2)]
            hn2k = [S.key("hn%d" % i) for i in range(2)]
            gmix = sb(st, "gmix", [128, D], F32)
            gmk = S.key("gmix")
            ss = [sb(st, "ss%d" % i, [128, 4], F32) for i in range(2)]
            ssk = [S.key("ss%d" % i) for i in range(2)]
            NPT = 6
            PT = [sb(st, "PT%d" % i, [128, 512], BF16) for i in range(NPT)]
            PTk = [S.key("PT%d" % i) for i in range(NPT)]
            validt = sb(st, "validt", [128, NW], F32)
            vk = S.key("valid")
            lamt = sb(st, "lamt", [128, 4, 64], F32)
            lamk = S.key("lam")
            lams = sb(st, "lams", [128, 8], F32)
            gsub = sb(st, "gsub", [128, 128], F32)
            gsk = S.key("gsub")
            vbias = sb(st, "vbias", [128, NW], F32)
            maskA = sb(st, "maskA", [128, 2, 2, 128], BF16)
            maskB = sb(st, "maskB", [128, 2, 2, 128], BF16)
            mkk = S.key("masks")
            save_ptr = mem["ptr"]
            mem["ptr"] = xs_off
            QTbd = [sb(st, "QTbd%d" % i, [128, 4, 2, 256], BF16) for i in range(1)] * 2
            QTbdk = [S.key("QTbd0")] * 2
            Zacc = [[sb(st, "Zacc%d_%d" % (p, q), [128, 512], F32) for q in range(2)] for p in range(2)]
            Zacck = [[S.key("Zacc%d_%d" % (p, q)) for q in range(2)] for p in range(2)]
            bufA = sb(st, "bufA", [128, 512], F32)
            bufB = sb(st, "bufB", [128, 256], F32)
            bufC = sb(st, "bufC", [128, 256], F32)
            bufAk, bufBk, bufCk = S.key("bufA"), S.key("bufB"), S.key("bufC")
            assert mem["ptr"] <= xs_off + 2 * D * 4
            mem["ptr"] = save_ptr
            gsubc = sb(st, "gsubc", [128, 1], F32)
            ones1 = sb(st, "ones1", [128, 128], F32)

            if DBG_MEM:
                print("stage1 mem ptr", mem["ptr"], "limit", mem["limit"], "free", mem["limit"] - mem["ptr"])
            S.dma("sp", gmix[:], bc(norm_mix_g, D), r=[], w=[gmk])
            S.dma("sp", validt[:], valid[:, :], r=[], w=[vk])
            for i, v in enumerate((lq1, lk1, lq2, lk2)):
                S.dma("sp", lamt[:, i, :], bc(v, 64), r=[], w=[lamk])
            S.dma("sp", gsub[:], bc(subln, 128), r=[], w=[gsk])
            S.dma("sp", gsubc[:], subln.rearrange("(p o) -> p o", o=1), r=[], w=[gsk])
            S.op("dve", lambda: V.tensor_scalar(out=gsubc[:], in0=gsubc[:], scalar1=(1.0 - LAM_INIT), scalar2=None, op0=ALU.mult), r=[gsk], w=[gsk])
            S.op("dve", lambda: V.tensor_scalar(out=vbias[:], in0=validt[:], scalar1=-1.0, scalar2=30000.0, op0=ALU.add, op1=ALU.mult), r=[vk], w=[vk])
            S.op("dve", lambda: V.memset(ones1[:], 1.0), w=[mkk])
            for c_ in range(2):
                S.op("dve", lambda: V.tensor_copy(out=maskA[:, c_, 0, :], in_=tri[:]), r=[kc], w=[mkk])
                S.op("dve", lambda: V.tensor_copy(out=maskA[:, c_, 1, :], in_=onesb[:]), r=[kc], w=[mkk])
                S.op("dve", lambda: V.tensor_copy(out=maskB[:, c_, 0, :], in_=zer[:, 0:128]), r=[kc], w=[mkk])
                S.op("dve", lambda: V.tensor_copy(out=maskB[:, c_, 1, :], in_=tri[:]), r=[kc], w=[mkk])
            S.op("dve", lambda: V.tensor_tensor(out=lamt[:, 0, :], in0=lamt[:, 0, :], in1=lamt[:, 1, :], op=ALU.mult), r=[lamk], w=[lamk])
            S.op("dve", lambda: V.tensor_tensor(out=lamt[:, 2, :], in0=lamt[:, 2, :], in1=lamt[:, 3, :], op=ALU.mult), r=[lamk], w=[lamk])
            S.op("dve", lambda: V.reduce_sum(out=lams[:, 0:1], in_=lamt[:, 0, :], axis=AX.X), r=[lamk], w=[lamk])
            S.op("dve", lambda: V.reduce_sum(out=lams[:, 1:2], in_=lamt[:, 2, :], axis=AX.X), r=[lamk], w=[lamk])
            S.op("act", lambda: A.activation(out=lams[:, 2:4], in_=lams[:, 0:2], func=AF.Exp), r=[lamk], w=[lamk])
            S.op("dve", lambda: V.tensor_tensor(out=lams[:, 4:5], in0=lams[:, 2:3], in1=lams[:, 3:4], op=ALU.subtract), r=[lamk], w=[lamk])
            S.op("dve", lambda: V.tensor_scalar(out=lams[:, 5:6], in0=lams[:, 4:5], scalar1=LAM_INIT, scalar2=None, op0=ALU.add), r=[lamk], w=[lamk])
            S.op("dve", lambda: V.tensor_scalar(out=gsub[:], in0=gsub[:], scalar1=(1.0 - LAM_INIT), scalar2=None, op0=ALU.mult), r=[gsk], w=[gsk])
            lam_ap = lams[:, 5:6]

            tcount = 0
            wcount = 0
            for hh in range(2):
                qc0 = 2048 + 512 * hh
                kc0 = 3072 + 512 * hh
                vc0 = 4096 + 512 * hh
                wblock(Wb[0], Wbk[0], w_in, kc0)
                wblock(Wb[1], Wbk[1], w_in, vc0)
                wblock(Wb[2], Wbk[2], w_in, qc0)
                def stage1_A(w_):
                    u_ = w_ % 2
                    S.dma("sp", xs[u_][:], xw[w_ * 128:(w_ + 1) * 128, :], r=[], w=[xsk[u_]])
                    rms_A(xs[u_][:], xsk[u_], gmix, gmk, hn2[u_], hn2k[u_], ss[u_], ssk[u_])

                def mm_group(grp):
                    hT, hTk = hnT[grp % 2], hnTk[grp % 2]
                    for j in range(4):
                        ps, pk = proj_featT(Wb[0], Wbk[0], j * 128, hT, hTk, 0, 256, None, None)
                        evac(KT[:, j, grp * 256:(grp + 1) * 256], ps[:, 0:256], r=[pk], w=[KTk])
                    for t in range(2):
                        ps, pk = proj_tok(Wb[1], Wbk[1], 512, hT, hTk, t * 128)
                        evac(VA[:, grp * 2 + t, :, :], ps[:, 0:512].rearrange("p (h d) -> p h d", h=4), r=[pk], w=[VAk])
                    if grp >= 12:
                        for j in range(4):
                            ps, pk = proj_featT(Wb[2], Wbk[2], j * 128, hT, hTk, 0, 256, None, None)
                            evac(QT[:, j, (grp - 12) * 256:(grp - 11) * 256], ps[:, 0:256], r=[pk], w=[QTk])

                stage1_A(0)
                for grp in range(16):
                    hT, hTk = hnT[grp % 2], hnTk[grp % 2]
                    for t in range(2):
                        w_ = grp * 2 + t
                        if w_ + 1 < NW:
                            stage1_A(w_ + 1)
                        rms_B(hn2[w_ % 2], hn2k[w_ % 2], hT, hTk, t * 128, evac_alt=t)
                    if grp >= 1:
                        mm_group(grp - 1)
                mm_group(15)

                S.op("pool", lambda: G.memset(QTbd[0][:], 0.0), r=[QTk], w=[QTbdk[0]])
                TPf = [TP[0][:, :].bitcast(F32), TP[1][:, :].bitcast(F32)]
                Sbanks = [(PS[4][:, 0:512], PSK[4]), (PS[5][:, 0:512], PSK[5]), (TPf[0], TPK[0])]
                zb, zbk = TPf[1], TPK[1]
                OTb = [[PS[0], PS[1]], [PS[2], PS[3]]]
                OTbk = [[PSK[0], PSK[1]], [PSK[2], PSK[3]]]

                def norm_steps(par, hl, g):
                    steps = []
                    for qh in range(2):
                        def s1(qh=qh):
                            ot_, otk_ = OTb[par][qh], OTbk[par][qh]
                            S.op("pe", lambda: PE.matmul(zb, lhsT=ones1[:], rhs=Zacc[par][qh][:], start=True, stop=True), r=[Zacck[par][qh], mkk], w=[zbk])
                            S.op("dve", lambda: V.reciprocal(out=bufA[:], in_=zb), r=[zbk], w=[bufAk])
                            S.op("dve", lambda: V.tensor_scalar(out=bufA[:, 256:512], in0=bufA[:, 256:512], scalar1=lam_ap, scalar2=None, op0=ALU.mult), r=[bufAk, lamk], w=[bufAk])
                            S.op("dve", lambda: V.tensor_tensor(out=bufA[:], in0=ot_[:, 0:512], in1=bufA[:], op=ALU.mult), r=[otk_, bufAk], w=[bufAk])
                            S.op("dve", lambda: V.tensor_tensor(out=bufA[:, 0:256], in0=bufA[:, 0:256], in1=bufA[:, 256:512], op=ALU.subtract), r=[bufAk], w=[bufAk])
                            S.op("act", lambda: A.activation(out=bufB[:], in_=bufA[:, 0:256], func=AF.Square), r=[bufAk], w=[bufBk])

                        def s2(qh=qh):
                            S.op("pe", lambda: PE.matmul(zb[:, 0:256], lhsT=onesf[:], rhs=bufB[:], start=True, stop=True), r=[bufBk, kc], w=[zbk])
                            S.op("act", lambda: A.activation(out=bufC[:], in_=zb[:, 0:256], func=AF.Ln, bias=epsr[:, 0:1], scale=1.0), r=[zbk, kc], w=[bufCk])
                            S.op("act", lambda: A.activation(out=bufC[:], in_=bufC[:], func=AF.Exp, scale=-0.5), r=[bufCk], w=[bufCk])
                            q0 = g * 512 + qh * 256
                            S.op("dve", lambda: V.scalar_tensor_tensor(out=attnT[:, 4 * hh + hl, q0:q0 + 256], in0=bufA[:, 0:256], scalar=gsubc[:, 0:1], in1=bufC[:], op0=ALU.mult, op1=ALU.mult), r=[bufAk, bufCk, gsk], w=[attnTk])

                        steps += [s1, s2]
                    return steps

                srr = 0
                blk = 0
                pending = None
                for hl in range(4):
                    qb, qbk = QTbd[hl % 2], QTbdk[hl % 2]
                    S.op("act", lambda: A.copy(out=qb[0:64, :, 0, :], in_=QT[0:64, hl, :].rearrange("p (g q) -> p g q", g=4)), r=[QTk], w=[qbk])
                    S.op("act", lambda: A.copy(out=qb[64:128, :, 1, :], in_=QT[64:128, hl, :].rearrange("p (g q) -> p g q", g=4)), r=[QTk], w=[qbk])
                    for g in range(2):
                        par = blk % 2
                        blk += 1
                        nkt = 24 + 4 * g + 4
                        units = []
                        for kt in range(nkt):
                            idiag = kt - (24 + 4 * g)
                            for qh in range(2):
                                if idiag > 2 * qh + 1:
                                    continue
                                units.append((kt, qh, idiag))
                        LAG = 2
                        slots = {}
                        for idx in range(len(units) + LAG):
                            if idx < len(units):
                                kt, qh, idiag = units[idx]
                                sp_, spk = Sbanks[srr % 3]
                                pt, ptk = PT[srr % NPT], PTk[srr % NPT]
                                slots[idx] = (pt, ptk)
                                srr += 1
                                S.op("pe", lambda: PE.matmul(sp_, lhsT=KT[:, hl, kt * 128:(kt + 1) * 128], rhs=qb[:, 2 * g + qh, :, :].rearrange("p c q -> p (c q)"), start=True, stop=True), r=[KTk, qbk], w=[spk])
                                S.op("act", lambda: A.activation(out=pt[:], in_=sp_, func=AF.Exp, bias=vbias[:, kt:kt + 1], scale=0.125), r=[spk, vk], w=[ptk])
                                if idiag == 2 * qh:
                                    S.op("pool", lambda: G.tensor_tensor(out=pt[:], in0=pt[:], in1=maskA[:].rearrange("p c j q -> p (c j q)"), op=ALU.mult), r=[ptk, mkk], w=[ptk])
                                elif idiag == 2 * qh + 1:
                                    S.op("pool", lambda: G.tensor_tensor(out=pt[:], in0=pt[:], in1=maskB[:].rearrange("p c j q -> p (c j q)"), op=ALU.mult), r=[ptk, mkk], w=[ptk])
                                za, zak = Zacc[par][qh], Zacck[par][qh]
                                if kt == 0:
                                    S.op("dve", lambda: V.tensor_copy(out=za[:], in_=pt[:]), r=[ptk], w=[zak])
                                elif kt % 5 < 3:
                                    S.op("dve", lambda: V.tensor_tensor(out=za[:], in0=za[:], in1=pt[:], op=ALU.add), r=[ptk, zak], w=[zak])
                                else:
                                    S.op("pool", lambda: G.tensor_tensor(out=za[:], in0=za[:], in1=pt[:], op=ALU.add), r=[ptk, zak], w=[zak])
                            if idx - LAG >= 0:
                                kt, qh, idiag = units[idx - LAG]
                                pt, ptk = slots.pop(idx - LAG)
                                klast = 24 + 4 * g + 2 * qh + 1
                                S.op("pe", lambda: PE.matmul(OTb[par][qh][:, 0:512], lhsT=VA[:, kt, hl, :], rhs=pt[:], start=(kt == 0), stop=(kt == klast)), r=[ptk, VAk], w=[OTbk[par][qh]])
                            if pending and idx in (6, 14, 22, 30):
                                pending.pop(0)()
                        assert not pending
                        pending = norm_steps(par, hl, g)
                while pending:
                    pending.pop(0)()
                S._deps("sp", [], [QTbdk[0], bufAk, bufBk, bufCk] + [Zacck[p_][q_] for p_ in range(2) for q_ in range(2)], dma=True)
            S.barrier()

        convT = sb(es, "convT", [128, 8, NT * 128], BF16)
        convTk = S.key("convT")
        with Scope() as st:
            NTK = 9 * 128
            hn9 = sb(st, "hn9", [128, NCH, NTK], BF16)
            hn9k = S.key("hn9")
            uT = sb(st, "uT", [128, 8, NTK], BF16)
            uTk = S.key("uT")
            Wb = [sb(st, "cWb%d" % i, [128, NCH, 512], BF16) for i in range(2)]
            Wbk = [S.key("cWb%d" % i) for i in range(2)]
            xs = [sb(st, "cxs%d" % i, [128, D], F32) for i in range(2)]
            xsk = [S.key("cxs%d" % i) for i in range(2)]
            hn = [sb(st, "chn%d" % i, [128, D], BF16) for i in range(2)]
            hnk = [S.key("chn%d" % i) for i in range(2)]
            gmix = sb(st, "cgmix", [128, D], F32)
            gmk = S.key("cgmix")
            ss = [sb(st, "css%d" % i, [128, 4], F32) for i in range(2)]
            ssk = [S.key("css%d" % i) for i in range(2)]
            wdw_nat = sb(st, "wdw_nat", [31, 1024], F32)
            wdwT = sb(st, "wdwT", [128, 8, 31], F32)
            cpar = sb(st, "cpar", [128, 3, 8], F32)
            cpk = S.key("cpar")
            Dg2 = [sb(st, "Dg%d" % i, [128, 31, 128], BF16) for i in range(2)]
            Dg2k = [S.key("Dg%d" % i) for i in range(2)]
            sg = [sb(st, "sg%d" % i, [128, 384], F32) for i in range(2)]
            sgk = [S.key("sg%d" % i) for i in range(2)]
            yf2 = [sb(st, "yf%d" % i, [128, 512], F32) for i in range(2)]
            yc2 = [sb(st, "yc%d" % i, [128, 512], F32) for i in range(2)]
            sq2 = [sb(st, "sq%d" % i, [128, 512], F32) for i in range(2)]
            sd2 = [sb(st, "sd%d" % i, [128, 512], F32) for i in range(2)]
            yf2k = [S.key("yf%d" % i) for i in range(2)]
            yc2k = [S.key("yc%d" % i) for i in range(2)]
            sq2k = [S.key("sq%d" % i) for i in range(2)]
            sd2k = [S.key("sd%d" % i) for i in range(2)]

            S.dma("sp", gmix[:], bc(norm_mix_g, D), r=[], w=[gmk])
            S.dma("sp", wdw_nat[:], conv_dw_w[:, :], r=[], w=[cpk])
            with nc.allow_non_contiguous_dma(reason="tiny per-channel parameter vectors"):
                for i, v in enumerate((conv_dw_b, conv_ln_g, conv_ln_b)):
                    S.dma("sp", cpar[:, i, :], v.rearrange("(c p) -> p c", p=128), r=[], w=[cpk])
            for cc in range(8):
                ps, pk = next_ps()
                S.op("pe", lambda: PE.transpose(out=ps[:, 0:31], in_=wdw_nat[:, cc * 128:(cc + 1) * 128], identity=identf[0:31, 0:31]), r=[cpk, kc], w=[pk])
                evac(wdwT[:, cc, :], ps[:, 0:31], r=[pk], w=[cpk])
            def conv_src(t):
                w_ = 23 + t
                S.dma("sp", xs[t % 2][:], xw[w_ * 128:(w_ + 1) * 128, :], r=[], w=[xsk[t % 2]])
                return xs[t % 2][:], xsk[t % 2]

            rms_loop(9, conv_src, gmix, gmk, hn, hnk, ss, ssk, hn9, hn9k)
            wcount = 0
            for bl in range(2):
                Wa, Wak = Wb[0], Wbk[0]
                Wg, Wgk = Wb[1], Wbk[1]
                wblock(Wa, Wak, w_in, bl * 512)
                wblock(Wg, Wgk, w_in, 1024 + bl * 512)
                for j in range(4):
                    cc = bl * 4 + j
                    for rr in range(3):
                        t0 = rr * 384
                        psa, pka = proj_featT(Wa, Wak, j * 128, hn9, hn9k, t0, 384, None, None)
                        psg, pkg = proj_featT(Wg, Wgk, j * 128, hn9, hn9k, t0, 384, None, None)
                        s_, s_k = sg[rr % 2], sgk[rr % 2]
                        S.op("act", lambda: A.activation(out=s_[:], in_=psg[:, 0:384], func=AF.Sigmoid), r=[pkg], w=[s_k])
                        S.op("dve", lambda: V.tensor_tensor(out=uT[:, cc, t0:t0 + 384], in0=psa[:, 0:384], in1=s_[:], op=ALU.mult), r=[pka, s_k], w=[uTk])
            def build_Dg(cc):
                Dg, Dgk = Dg2[cc % 2], Dg2k[cc % 2]
                for j in range(31):
                    S.op("dve", lambda j=j: V.tensor_scalar(out=Dg[:, j, :], in0=ident[:], scalar1=wdwT[:, cc, j:j + 1], scalar2=None, op0=ALU.mult), r=[cpk, kc], w=[Dgk])

            def conv_T(u):
                cc, half = divmod(u, 2)
                Dg, Dgk = Dg2[cc % 2], Dg2k[cc % 2]
                ps, pk = next_ps()
                for j in range(31):
                    o = 128 + half * 512 - 30 + j
                    S.op("pe", lambda j=j, o=o: PE.matmul(ps[:, 0:512], lhsT=Dg[:, j, :], rhs=uT[:, cc, o:o + 512], start=(j == 0), stop=(j == 30)), r=[Dgk, uTk], w=[pk])
                if half == 0 and cc + 1 < 8:
                    build_Dg(cc + 1)
                return ps, pk

            def conv_LN(u, ps, pk):
                cc, half = divmod(u, 2)
                v_ = u % 2
                yf, yc, sq, sd = yf2[v_], yc2[v_], sq2[v_], sd2[v_]
                yfk, yck, sqk, sdk = yf2k[v_], yc2k[v_], sq2k[v_], sd2k[v_]
                S.op("act", lambda: A.activation(out=yf[:], in_=ps[:, 0:512], func=AF.Identity, bias=cpar[:, 0, cc:cc + 1], scale=1.0), r=[pk, cpk], w=[yfk])
                pm, pmk = next_ps()
                S.op("pe", lambda: PE.matmul(pm[:, 0:512], lhsT=onesf[:], rhs=yf[:], start=True, stop=True), r=[yfk, kc], w=[pmk])
                S.op("dve", lambda: V.tensor_tensor(out=yc[:], in0=yf[:], in1=pm[:, 0:512], op=ALU.subtract), r=[yfk, pmk], w=[yck])
                S.op("act", lambda: A.activation(out=sq[:], in_=yc[:], func=AF.Square), r=[yck], w=[sqk])
                pv, pvk = next_ps()
                S.op("pe", lambda: PE.matmul(pv[:, 0:512], lhsT=onesf[:], rhs=sq[:], start=True, stop=True), r=[sqk, kc], w=[pvk])
                S.op("act", lambda: A.activation(out=sd[:], in_=pv[:, 0:512], func=AF.Ln, bias=epsl[:, 0:1], scale=1.0), r=[pvk, kc], w=[sdk])
                S.op("act", lambda: A.activation(out=sd[:], in_=sd[:], func=AF.Exp, scale=-0.5), r=[sdk], w=[sdk])
                S.op("dve", lambda: V.tensor_tensor(out=yc[:], in0=yc[:], in1=sd[:], op=ALU.mult), r=[yck, sdk], w=[yck])
                S.op("act", lambda: A.activation(out=convT[:, cc, half * 512:(half + 1) * 512], in_=yc[:], func=AF.Silu, bias=cpar[:, 2, cc:cc + 1], scale=cpar[:, 1, cc:cc + 1]), r=[yck, cpk], w=[convTk])

            build_Dg(0)
            prev = conv_T(0)
            for u in range(16):
                nxt = conv_T(u + 1) if u + 1 < 16 else None
                conv_LN(u, *prev)
                prev = nxt
            S.barrier()

        h = nc.alloc_sbuf_tensor_at("h_res", [128, NT, D], F32, offset=H_OFF)
        mem["limit"] = H_OFF
        hk = [S.key("h%d" % i) for i in range(NT)]
        for i in range(NT):
            S.dma("sp", h[:, i, :], xw[(24 + i) * 128:(25 + i) * 128, :], r=[], w=[hk[i]])

        def proj_residual(st, wdram, srcs, tag, Wb=None, Wbk=None):
            if Wb is None:
                Wb = [sb(st, "%sWb%d" % (tag, i), [128, NCH, 512], BF16) for i in range(2)]
                Wbk = [S.key("%sWb%d" % (tag, i)) for i in range(2)]
            for nb in range(4):
                Wt, Wk = Wb[nb % 2], Wbk[nb % 2]
                wblock(Wt, Wk, wdram, nb * 512)
                for i in range(NT):
                    ps, pk = next_ps()
                    for c in range(NCH):
                        tns, ci, tk = srcs[c]
                        S.op("pe", lambda c=c, tns=tns, ci=ci: PE.matmul(ps[:, 0:512], lhsT=tns[:, ci, i * 128:(i + 1) * 128], rhs=Wt[:, c, :], start=(c == 0), stop=(c == NCH - 1)), r=[Wk, tk], w=[pk])
                    S.op("dve", lambda: V.tensor_tensor(out=h[:, i, nb * 512:(nb + 1) * 512], in0=h[:, i, nb * 512:(nb + 1) * 512], in1=ps[:, 0:512], op=ALU.add), r=[pk, hk[i]], w=[hk[i]])

        with Scope() as st:
            srcs = [(convT, c, convTk) for c in range(8)] + [(attnT, c, attnTk) for c in range(8)]
            proj_residual(st, w_out, srcs, "o")
            S.barrier()
        mem["ptr"] = mark_mix

        def dump(idx):
            if DBG:
                dk = S.key("dbg%d" % idx)
                for i in range(NT):
                    S.dma("sp", dbg[idx, i * 128:(i + 1) * 128, :], h[:, i, :], r=[hk[i]], w=[dk])

        dump(0)

        with Scope() as st:
            hcT = sb(st, "hcT", [128, NCH, NT * 128], BF16)
            hcTk = S.key("hcT")
            KcT = sb(st, "KcT", [128, NCH, 256], BF16)
            KcTk = S.key("KcT")
            Vc = sb(st, "Vc", [128, 2, D], BF16)
            Vck = S.key("Vc")
            Wb = [sb(st, "xWb%d" % i, [128, NCH, 512], BF16) for i in range(2)]
            Wbk = [S.key("xWb%d" % i) for i in range(2)]
            hn = [sb(st, "xhn%d" % i, [128, D], BF16) for i in range(2)]
            hnk = [S.key("xhn%d" % i) for i in range(2)]
            ss = [sb(st, "xss%d" % i, [128, 4], F32) for i in range(2)]
            ssk = [S.key("xss%d" % i) for i in range(2)]
            memscope = Scope()
            memscope.__enter__()
            xs = [sb(st, "xxs%d" % i, [128, D], F32) for i in range(2)]
            xsk = [S.key("xxs%d" % i) for i in range(2)]
            gme = sb(st, "gme", [128, D], F32)
            gmek = S.key("gme")
            mnT = sb(st, "mnT", [128, NCH, 256], BF16)
            mnTk = S.key("mnT")

            S.dma("sp", gme[:], bc(norm_mem_g, D), r=[], w=[gmek])
            for t in range(2):
                S.dma("sp", xs[t][:], memb[t * 128:(t + 1) * 128, :], r=[], w=[xsk[t]])
                rms_to_T(st, xs[t][:], xsk[t], gme, gmek, hn[t], hnk[t], hn[t], ss[t], ssk[t], mnT, mnTk, t * 128, evac_alt=t)
            wcount = 0
            for nb in range(4):
                Wt, Wk = Wb[wcount % 2], Wbk[wcount % 2]
                wcount += 1
                wblock(Wt, Wk, w_ckv, nb * 512)
                for j in range(4):
                    ps, pk = proj_featT(Wt, Wk, j * 128, mnT, mnTk, 0, 256, None, None)
                    evac(KcT[:, nb * 4 + j, :], ps[:, 0:256], r=[pk], w=[KcTk])
            for nb in range(4):
                Wt, Wk = Wb[wcount % 2], Wbk[wcount % 2]
                wcount += 1
                wblock(Wt, Wk, w_ckv, 2048 + nb * 512)
                for mt in range(2):
                    ps, pk = proj_tok(Wt, Wk, 512, mnT, mnTk, mt * 128)
                    evac(Vc[:, mt, nb * 512:(nb + 1) * 512], ps[:, 0:512], r=[pk], w=[Vck])
            S.barrier()
            memscope.__exit__(None, None, None)
            gcr = sb(st, "gcr", [128, D], F32)
            gcrk = S.key("gcr")
            QcT = sb(st, "QcT", [128, NCH, NT * 128], BF16)
            QcTk = S.key("QcT")
            PcT = [sb(st, "PcT%d" % i, [128, 512], BF16) for i in range(2)]
            PcTk = [S.key("PcT%d" % i) for i in range(2)]
            PcN = [sb(st, "PcN%d" % i, [128, 512], BF16) for i in range(2)]
            PcNk = [S.key("PcN%d" % i) for i in range(2)]
            rZ = sb(st, "rZ", [128, 512], F32)
            rZk = S.key("rZ")
            S.dma("sp", gcr[:], bc(norm_cross_g, D), r=[], w=[gcrk])
            rms_loop(NT, lambda i: (h[:, i, :], hk[i]), gcr, gcrk, hn, hnk, ss, ssk, hcT, hcTk)
            for nb in range(4):
                Wt, Wk = Wb[wcount % 2], Wbk[wcount % 2]
                wcount += 1
                wblock(Wt, Wk, w_cq, nb * 512)
                for j in range(4):
                    for half in range(2):
                        ps, pk = proj_featT(Wt, Wk, j * 128, hcT, hcTk, half * 512, 512, None, None)
                        evac(QcT[:, nb * 4 + j, half * 512:(half + 1) * 512], ps[:, 0:512], r=[pk], w=[QcTk])
            ocT, ocTk = hcT, hcTk
            sc = 512.0 ** -0.5
            for hd in range(4):
                for half in range(2):
                    for mt in range(2):
                        ps, pk = next_ps()
                        for cc in range(4):
                            S.op("pe", lambda cc=cc: PE.matmul(ps[:, 0:512], lhsT=KcT[:, 4 * hd + cc, mt * 128:(mt + 1) * 128], rhs=QcT[:, 4 * hd + cc, half * 512:(half + 1) * 512], start=(cc == 0), stop=(cc == 3)), r=[KcTk, QcTk], w=[pk])
                        S.op("act", lambda: A.activation(out=PcT[mt][:], in_=ps[:, 0:512], func=AF.Exp, scale=sc), r=[pk], w=[PcTk[mt]])
                    pz, pzk = next_ps()
                    for mt in range(2):
                        S.op("pe", lambda mt=mt: PE.matmul(pz[:, 0:512], lhsT=onesb[:], rhs=PcT[mt][:], start=(mt == 0), stop=(mt == 1)), r=[PcTk[mt], kc], w=[pzk])
                    S.op("dve", lambda: V.reciprocal(out=rZ[:], in_=pz[:, 0:512]), r=[pzk], w=[rZk])
                    for mt in range(2):
                        S.op("dve", lambda mt=mt: V.tensor_tensor(out=PcN[mt][:], in0=PcT[mt][:], in1=rZ[:], op=ALU.mult), r=[PcTk[mt], rZk], w=[PcNk[mt]])
                    for cc in range(4):
                        po, pok = next_ps()
                        for mt in range(2):
                            S.op("pe", lambda mt=mt, cc=cc: PE.matmul(po[:, 0:512], lhsT=Vc[:, mt, hd * 512 + cc * 128:hd * 512 + (cc + 1) * 128], rhs=PcN[mt][:], start=(mt == 0), stop=(mt == 1)), r=[Vck, PcNk[mt]], w=[pok])
                        evac(ocT[:, 4 * hd + cc, half * 512:(half + 1) * 512], po[:, 0:512], r=[pok], w=[ocTk])
            S.barrier()
            proj_residual(st, w_co, [(ocT, c, ocTk) for c in range(NCH)], "c", Wb, Wbk)
            S.barrier()
        dump(1)

        with Scope() as st:
            with Scope() as st1:
                v12a = sb(st1, "v12a", [128, NT, 2, 8, 16], F32)
                v12ak = S.key("v12a")
                scva = sb(st1, "scva", [128, NT, 8, 16], F32)
                scvak = S.key("scva")
                rZa = sb(st1, "rZa", [128, NT, 8], F32)
                rZak = S.key("rZa")
                sdk3 = [S.key("sdram%d" % i) for i in range(3)]

                def ps6():
                    i6 = ps_rr[0] % 6
                    ps_rr[0] += 1
                    return PS[i6], PSK[i6]

                with Scope() as st0:
                    hpT = sb(st0, "hpT", [128, NCH, NT * 128], BF16)
                    hpTk = S.key("hpT")
                    KpT = [sb(st0, "KpT%d" % i, [128, 8, 128], F32) for i in range(2)]
                    KpTk = [S.key("KpT%d" % i) for i in range(2)]
                    with Scope() as st00:
                        hn = [sb(st00, "phn%d" % i, [128, D], BF16) for i in range(2)]
                        hnk = [S.key("phn%d" % i) for i in range(2)]
                        gpe = sb(st00, "gpe", [128, D], F32)
                        gpek = S.key("gpe")
                        ss = [sb(st00, "pss%d" % i, [128, 4], F32) for i in range(2)]
                        ssk = [S.key("pss%d" % i) for i in range(2)]
                        knat = sb(st00, "knat", [128, 8, 128], F32)
                        knk = S.key("knat")
                        S.dma("sp", gpe[:], bc(norm_peer_g, D), r=[], w=[gpek])
                        rms_loop(NT, lambda i: (h[:, i, :], hk[i]), gpe, gpek, hn, hnk, ss, ssk, hpT, hpTk)
                        for si, kd in enumerate((keys1, keys2)):
                            S.dma("sp", knat[:], kd.rearrange("h n d -> n h d"), r=[], w=[knk])
                            for hh_ in range(8):
                                ps, pk = next_ps()
                                S.op("pe", lambda: PE.transpose(out=ps[:, 0:128], in_=knat[:, hh_, :], identity=identf[:]), r=[knk, kc], w=[pk])
                                evac(KpT[si][:, hh_, :], ps[:, 0:128], r=[pk], w=[KpTk[si]])
                        S.barrier()
                    Wb = [sb(st0, "pWb%d" % i, [128, NCH, 256], BF16) for i in range(2)]
                    Wbk = [S.key("pWb%d" % i) for i in range(2)]
                    Qh = [[sb(st0, "Qh%d_%d" % (p, j), [128, NT * 128], F32) for j in range(2)] for p in range(2)]
                    Qhk = [[S.key("Qh%d_%d" % (p, j)) for j in range(2)] for p in range(2)]
                    sst = [sb(st0, "sst%d" % i, [128, 2, 128], F32) for i in range(3)]
                    sstk = [S.key("sst%d" % i) for i in range(3)]
                    swk = [sb(st0, "swk%d" % i, [128, 128], F32) for i in range(2)]
                    swkk = [S.key("swk%d" % i) for i in range(2)]
                    cand = [sb(st0, "cand%d" % i, [128, 256], F32) for i in range(2)]
                    candk = [S.key("cand%d" % i) for i in range(2)]
                    esc = sb(st0, "esc", [128, NT * 8, 16], F32)
                    esck = S.key("esc")
                    zsum = sb(st0, "zsum", [128, NT * 8], F32)
                    un = 0
                    for nb in range(8):
                        Wt, Wk = Wb[nb % 2], Wbk[nb % 2]
                        wblock(Wt, Wk, w_pq, nb * 256, ncols=256)
                        for j in range(2):
                            for half in range(2):
                                ps, pk = ps6()
                                for c in range(NCH):
                                    S.op("pe", lambda c=c: PE.matmul(ps[:, 0:512], lhsT=Wt[:, c, j * 128:(j + 1) * 128], rhs=hpT[:, c, half * 512:(half + 1) * 512], start=(c == 0), stop=(c == NCH - 1)), r=[Wk, hpTk], w=[pk])
                                S.op("act", lambda: A.copy(out=Qh[nb % 2][j][:, half * 512:(half + 1) * 512], in_=ps[:, 0:512]), r=[pk], w=[Qhk[nb % 2][j]])
                        for i in range(NT):
                            st_, stk_ = sst[un % 3], sstk[un % 3]
                            sw_, swk_ = swk[un % 2], swkk[un % 2]
                            cd_, cdk_ = cand[un % 2], candk[un % 2]
                            un += 1
                            ps, pk = ps6()
                            for side in range(2):
                                S.op("pe", lambda: PE.matmul(ps[:, side * 128:(side + 1) * 128], lhsT=Qh[nb % 2][side][:, i * 128:(i + 1) * 128], rhs=KpT[side][:, nb, :], start=True, stop=True), r=[Qhk[nb % 2][side], KpTk[side]], w=[pk])
                            S.op("act", lambda: A.copy(out=st_[:], in_=ps[:, 0:256].rearrange("p (s n) -> p s n", s=2)), r=[pk], w=[stk_])
                            S.dma("sp", sdram[i, :, :, nb, :], st_[:], r=[stk_], w=[sdk3[(un - 1) % 3]])
                            for side in range(2):
                                vX = v12a[:, i, side, nb, :]
                                S.op("dve", lambda: V.max(out=vX[:, 0:8], in_=st_[:, side, :]), r=[stk_], w=[v12ak])
                                S.op("dve", lambda: V.match_replace(out=sw_[:], in_to_replace=vX[:, 0:8], in_values=st_[:, side, :], imm_value=-1e30), r=[stk_, v12ak], w=[swk_])
                                S.op("dve", lambda: V.max(out=vX[:, 8:16], in_=sw_[:]), r=[swk_], w=[v12ak])
                            S.op("dve", lambda: V.tensor_tensor(out=cd_[:].rearrange("p (i j) -> p i j", i=16), in0=v12a[:, i, 0, nb, :].unsqueeze(2).to_broadcast([128, 16, 16]), in1=v12a[:, i, 1, nb, :].unsqueeze(1).to_broadcast([128, 16, 16]), op=ALU.add), r=[v12ak], w=[cdk_])
                            sX = scva[:, i, nb, :]
                            S.op("dve", lambda: V.max(out=sX[:, 0:8], in_=cd_[:]), r=[cdk_], w=[scvak])
                            S.op("dve", lambda: V.match_replace(out=cd_[:], in_to_replace=sX[:, 0:8], in_values=cd_[:], imm_value=-1e30), r=[cdk_, scvak], w=[cdk_])
                            S.op("dve", lambda: V.max(out=sX[:, 8:16], in_=cd_[:]), r=[cdk_], w=[scvak])
                    scf = scva[:].rearrange("p i h r -> p (i h) r")
                    S.op("dve", lambda: V.tensor_tensor(out=esc[:], in0=scf, in1=scf[:, :, 0:1].to_broadcast([128, NT * 8, 16]), op=ALU.subtract), r=[scvak], w=[esck])
                    S.op("act", lambda: A.activation(out=esc[:], in_=esc[:], func=AF.Exp), r=[esck], w=[esck])
                    S.op("dve", lambda: V.reduce_sum(out=zsum[:], in_=esc[:], axis=AX.X), r=[esck], w=[esck])
                    S.op("dve", lambda: V.reciprocal(out=rZa[:].rearrange("p i h -> p (i h)"), in_=zsum[:]), r=[esck], w=[rZak])
                    S.barrier()

                s12 = [sb(st1, "s12_%d" % i, [128, 2, 8, 128], F32) for i in range(2)]
                s12k = [S.key("s12_%d" % i) for i in range(2)]
                sumQ2 = [sb(st1, "sumQ%d" % i, [128, 8, 16, 16], F32) for i in range(2)]
                sumQ2k = [S.key("sumQ%d" % i) for i in range(2)]
                tmb = [sb(st1, "tmb%d" % i, [128, 8, 16, 16], BF16) for i in range(3)]
                tmbk = [S.key("tmb%d" % i) for i in range(3)]
                rot = {"q": 0, "t": 0, "b": 0}
                Qk_ = sb(st1, "Qk", [128, 128, 128], BF16)
                Qkk = S.key("Qk")
                P1k = sb(st1, "P1k", [128, 128, 128], BF16)
                P1kk = S.key("P1k")
                Wall = sb(st1, "Wall", [128, 64, 128], BF16)
                Wallk = S.key("Wall")
                E2 = sb(st1, "E2", [128, 8, 128], BF16)
                E2k = S.key("E2")
                Af = sb(st1, "Af", [128, 8, 16], F32)
                Afk = S.key("Af")
                AT = sb(st1, "AT", [128, 128], F32)
                ATk = S.key("AT")
                wdk = S.key("wd")
                if DBG_MEM:
                    print("phase2 mem ptr", mem["ptr"], "limit", mem["limit"], "free", mem["limit"] - mem["ptr"])
                S.dma("sp", s12[0][:], sdram[0, :, :, :, :], r=sdk3, w=[s12k[0]])
                for tile_i in range(NT):
                    if tile_i + 1 < NT:
                        S.dma("sp", s12[(tile_i + 1) % 2][:], sdram[tile_i + 1, :, :, :, :], r=sdk3, w=[s12k[(tile_i + 1) % 2]])
                    sk_ = s12k[tile_i % 2]
                    s1, s2 = s12[tile_i % 2][:, 0, :, :], s12[tile_i % 2][:, 1, :, :]
                    v1, v2 = v12a[:, tile_i, 0, :, :], v12a[:, tile_i, 1, :, :]
                    S.op("dve", lambda: V.tensor_tensor(out=Af[:], in0=v1, in1=v1[:, :, 0:1].to_broadcast([128, 8, 16]), op=ALU.subtract), r=[v12ak], w=[Afk])
                    S.op("act", lambda: A.activation(out=Af[:], in_=Af[:], func=AF.Exp), r=[Afk], w=[Afk])
                    S.op("dve", lambda: V.tensor_tensor(out=Af[:], in0=Af[:], in1=rZa[:, tile_i, :].unsqueeze(2).to_broadcast([128, 8, 16]), op=ALU.mult), r=[Afk, rZak], w=[Afk])
                    ps, pk = ps6()
                    S.op("pe", lambda: PE.transpose(out=ps[:, 0:128], in_=Af[:].rearrange("p h i -> p (h i)"), identity=identf[:]), r=[Afk, kc], w=[pk])
                    S.op("act", lambda: A.copy(out=AT[:], in_=ps[:, 0:128]), r=[pk], w=[ATk])
                    sq0, sq0k = sumQ2[rot["q"] % 2], sumQ2k[rot["q"] % 2]
                    rot["q"] += 1
                    tmpE = sq0[:].rearrange("p h i b -> p (h i b)")[:, 0:1024].rearrange("p (h b) -> p h b", h=8)
                    S.op("dve", lambda: V.tensor_tensor(out=tmpE, in0=s2, in1=v2[:, :, 0:1].to_broadcast([128, 8, 128]), op=ALU.subtract), r=[sk_, v12ak], w=[sq0k])
                    S.op("act", lambda: A.activation(out=E2[:], in_=tmpE, func=AF.Exp), r=[sq0k], w=[E2k])
                    for r8 in range(8):
                        sq, sqk = sumQ2[rot["q"] % 2], sumQ2k[rot["q"] % 2]
                        rot["q"] += 1
                        tb, tbk = tmb[rot["t"] % 3], tmbk[rot["t"] % 3]
                        rot["t"] += 1
                        S.op("pool", lambda: G.tensor_tensor(out=sq[:], in0=v1.unsqueeze(3).to_broadcast([128, 8, 16, 16]), in1=s2[:, :, r8 * 16:(r8 + 1) * 16].unsqueeze(2).to_broadcast([128, 8, 16, 16]), op=ALU.add), r=[v12ak, sk_], w=[sqk])
                        for hh_ in range(8):
                            S.op("dve", lambda: V.scalar_tensor_tensor(out=tb[:, hh_, :, :], in0=sq[:, hh_, :, :], scalar=scva[:, tile_i, hh_, 15:16], in1=E2[:, hh_, r8 * 16:(r8 + 1) * 16].unsqueeze(1).to_broadcast([128, 16, 16]), op0=ALU.is_ge, op1=ALU.mult), r=[sqk, scvak, E2k], w=[tbk])
                        Qv = tb[:].rearrange("p h i b -> p (h i) b")
                        for b8 in range(2):
                            bank, bankk = TP[rot["b"] % 2], TPK[rot["b"] % 2]
                            rot["b"] += 1
                            for bb in range(8):
                                S.op("pe", lambda: PE.transpose(out=bank[:, bb * 128:(bb + 1) * 128], in_=Qv[:, :, b8 * 8 + bb], identity=ident[:]), r=[tbk, kc], w=[bankk])
                            b0 = r8 * 16 + b8 * 8
                            S.op("act", lambda: A.copy(out=Qk_[:, :, b0:b0 + 8], in_=bank[:, :].rearrange("p (b t) -> p t b", b=8)), r=[bankk], w=[Qkk])
                        tb, tbk = tmb[rot["t"] % 3], tmbk[rot["t"] % 3]
                        rot["t"] += 1
                        S.op("dve", lambda: V.tensor_tensor(out=tb[:], in0=s1[:, :, r8 * 16:(r8 + 1) * 16].unsqueeze(2).to_broadcast([128, 8, 16, 16]), in1=v1.unsqueeze(3).to_broadcast([128, 8, 16, 16]), op=ALU.is_equal), r=[sk_, v12ak], w=[tbk])
                        Pv = tb[:].rearrange("p h i a -> p (h i) a")
                        for a8 in range(2):
                            bank, bankk = TP[rot["b"] % 2], TPK[rot["b"] % 2]
                            rot["b"] += 1
                            for aa in range(8):
                                S.op("pe", lambda: PE.transpose(out=bank[:, aa * 128:(aa + 1) * 128], in_=Pv[:, :, a8 * 8 + aa], identity=ident[:]), r=[tbk, kc], w=[bankk])
                            a0_ = r8 * 16 + a8 * 8
                            S.op("dve", lambda: V.tensor_tensor(out=P1k[:, :, a0_:a0_ + 8], in0=bank[:, :].rearrange("p (a t) -> p t a", a=8), in1=AT[:].unsqueeze(2).to_broadcast([128, 128, 8]), op=ALU.mult), r=[bankk, ATk], w=[P1kk])
                    for ah in range(2):
                        for t8 in range(16):
                            ps, pk = ps6()
                            for tt in range(8):
                                t_ = t8 * 8 + tt
                                S.op("pe", lambda: PE.matmul(ps[:, tt * 64:(tt + 1) * 64], lhsT=Qk_[:, t_, :], rhs=P1k[:, t_, ah * 64:(ah + 1) * 64], start=True, stop=True), r=[Qkk, P1kk], w=[pk])
                            S.op("act", lambda: A.copy(out=Wall[:, :, t8 * 8:(t8 + 1) * 8], in_=ps[:, 0:512].rearrange("p (t a) -> p a t", t=8)), r=[pk], w=[Wallk])
                        for a4 in range(4):
                            a0 = ah * 64 + a4 * 16
                            S.dma("sp", wd[a0:a0 + 16, :, tile_i * 128:(tile_i + 1) * 128].rearrange("a b t -> b a t"), Wall[:, a4 * 16:(a4 + 1) * 16, :], r=[Wallk], w=[wdk])
                S.barrier()

            with Scope() as st2:
                GRP = 4
                NSL = 3
                Uc = [sb(st2, "Uc%d" % i, [128, D], BF16) for i in range(NSL)]
                Uck = [S.key("Uc%d" % i) for i in range(NSL)]
                UT = [sb(st2, "UT%d" % i, [128, NCH, 128], BF16) for i in range(2)]
                UTk = [S.key("UT%d" % i) for i in range(2)]
                Vg = [[sb(st2, "Vg%d_%d" % (p, i), [128, D], BF16) for i in range(GRP)] for p in range(2)]
                Vgk = [[S.key("Vg%d_%d" % (p, i)) for i in range(GRP)] for p in range(2)]
                Wc = [sb(st2, "Wc%d" % i, [128, NT * 128], BF16) for i in range(NSL)]
                Wck = [S.key("Wc%d" % i) for i in range(NSL)]
                gl = [sb(st2, "gl%d" % i, [128, NT * 128], BF16) for i in range(2)]
                glk = [S.key("gl%d" % i) for i in range(2)]
                GH = [sb(st2, "GH%d" % i, [128, NT * 128], BF16) for i in range(2 * GRP)]
                GHk = [S.key("GH%d" % i) for i in range(2 * GRP)]
                Hb = (PS[4], PS[5])
                Hbk = (PSK[4], PSK[5])
                hpT = sb(st2, "hpT2", [128, NCH, NT * 128], BF16)
                hpTk = S.key("hpT2")
                with Scope() as st3:
                    hn = [sb(st3, "qhn%d" % i, [128, D], BF16) for i in range(2)]
                    hnk = [S.key("qhn%d" % i) for i in range(2)]
                    gpe = sb(st3, "qgpe", [128, D], F32)
                    gpek = S.key("qgpe")
                    ss = [sb(st3, "qss%d" % i, [128, 4], F32) for i in range(2)]
                    ssk = [S.key("qss%d" % i) for i in range(2)]
                    S.dma("sp", gpe[:], bc(norm_peer_g, D), r=[], w=[gpek])
                    rms_loop(NT, lambda i: (h[:, i, :], hk[i]), gpe, gpek, hn, hnk, ss, ssk, hpT, hpTk)
                    S.barrier()

                def prefetch(ch):
                    s = ch % NSL
                    S.dma("pool", Uc[s][:], peer_u[ch * 128:(ch + 1) * 128, :], r=[], w=[Uck[s]])
                    p, i_ = (ch // GRP) % 2, ch % GRP
                    S.dma("pool", Vg[p][i_][:], peer_v[ch * 128:(ch + 1) * 128, :], r=[], w=[Vgk[p][i_]])
                    S.dma("sp", Wc[s][:], wd[ch, :, :], r=[wdk], w=[Wck[s]])

                prefetch(0)
                prefetch(1)
                NCHK = 128

                def do_T(ch):
                    s_ = ch % NSL
                    ut, utk = UT[ch % 2], UTk[ch % 2]
                    for half in range(2):
                        for c in range(8):
                            S.op("pe", lambda c=c: PE.transpose(out=TP[half][:, c * 128:(c + 1) * 128], in_=Uc[s_][:, (half * 8 + c) * 128:(half * 8 + c + 1) * 128], identity=ident[:]), r=[Uck[s_], kc], w=[TPK[half]])
                        evac(ut[:, half * 8:(half + 1) * 8, :], TP[half][:, :].rearrange("p (c n) -> p c n", c=8), r=[TPK[half]], w=[utk])

                def do_H(ch):
                    s_ = ch % NSL
                    ut, utk = UT[ch % 2], UTk[ch % 2]
                    gh, ghk = GH[ch % (2 * GRP)], GHk[ch % (2 * GRP)]
                    for half in range(2):
                        for c in range(NCH):
                            S.op("pe", lambda c=c: PE.matmul(Hb[half][:, 0:512], lhsT=ut[:, c, :], rhs=hpT[:, c, half * 512:(half + 1) * 512], start=(c == 0), stop=(c == NCH - 1)), r=[utk, hpTk], w=[Hbk[half]])
                        S.op("act", lambda: A.activation(out=gl[ch % 2][:, half * 512:(half + 1) * 512], in_=Hb[half][:, 0:512], func=AF.Gelu), r=[Hbk[half]], w=[glk[ch % 2]])
                    S.op("dve", lambda: V.tensor_tensor(out=gh[:], in0=gl[ch % 2][:], in1=Wc[s_][:], op=ALU.mult), r=[glk[ch % 2], Wck[s_]], w=[ghk])

                def do_Y(gi):
                    p = gi % 2
                    for i in range(NT):
                        for nb in range(4):
                            for ci in range(GRP):
                                gh, ghk = GH[(gi * GRP + ci) % (2 * GRP)], GHk[(gi * GRP + ci) % (2 * GRP)]
                                S.op("pe", lambda ci=ci, nb=nb: PE.matmul(PS[nb][:, 0:512], lhsT=gh[:, i * 128:(i + 1) * 128], rhs=Vg[p][ci][:, nb * 512:(nb + 1) * 512], start=(ci == 0), stop=(ci == GRP - 1)), r=[ghk, Vgk[p][ci]], w=[PSK[nb]])
                            S.op("dve", lambda nb=nb: V.tensor_tensor(out=h[:, i, nb * 512:(nb + 1) * 512], in0=h[:, i, nb * 512:(nb + 1) * 512], in1=PS[nb][:, 0:512], op=ALU.add), r=[PSK[nb], hk[i]], w=[hk[i]])

                do_T(0)
                for ch in range(NCHK):
                    if ch + 2 < NCHK:
                        prefetch(ch + 2)
                    if ch + 1 < NCHK:
                        do_T(ch + 1)
                    do_H(ch)
                    if ch % GRP == 0 and ch >= GRP:
                        do_Y(ch // GRP - 1)
                do_Y(NCHK // GRP - 1)
                S.barrier()
        dump(2)

        with Scope() as st:
            gf = sb(st, "gf", [128, D], F32)
            gfk = S.key("gf")
            yo = [sb(st, "yo%d" % i, [128, D], F32) for i in range(2)]
            yok = [S.key("yo%d" % i) for i in range(2)]
            sqj = sb(st, "fsq", [128, D], BF16)
            sqk = S.key("fsq")
            ss = [sb(st, "fss%d" % i, [128, 4], F32) for i in range(2)]
            ssk = [S.key("fss%d" % i) for i in range(2)]
            yk2 = [S.key("y%d" % i) for i in range(2)]
            S.dma("sp", gf[:], bc(final_g, D), r=[], w=[gfk])
            for i in range(NT):
                u = i % 2
                S.op("act", lambda: A.activation(out=sqj[:], in_=h[:, i, :], func=AF.Square, accum_out=ss[u][:, 0:1]), r=[hk[i]], w=[sqk, ssk[u]])
                S.op("dve", lambda: V.tensor_scalar(out=ss[u][:, 1:2], in0=ss[u][:, 0:1], scalar1=1.0 / D, scalar2=RMS_EPS, op0=ALU.mult, op1=ALU.add), r=[ssk[u]], w=[ssk[u]])
                S.op("act", lambda: A.activation(out=ss[u][:, 2:3], in_=ss[u][:, 1:2], func=AF.Sqrt), r=[ssk[u]], w=[ssk[u]])
                S.op("dve", lambda: V.reciprocal(out=ss[u][:, 3:4], in_=ss[u][:, 2:3]), r=[ssk[u]], w=[ssk[u]])
                S.op("dve", lambda: V.scalar_tensor_tensor(out=yo[u][:], in0=h[:, i, :], scalar=ss[u][:, 3:4], in1=gf[:], op0=ALU.mult, op1=ALU.mult), r=[hk[i], ssk[u], gfk], w=[yok[u]])
                S.dma("sp", y_out[i * 128:(i + 1) * 128, :], yo[u][:], r=[yok[u]], w=[yk2[u]])
            S.barrier()
    return nc


_NC_CACHE = {}


def kernel(**inputs):
    x = np.ascontiguousarray(np.asarray(inputs["x"], dtype=np.float32))
    mem = np.ascontiguousarray(np.asarray(inputs["mem"], dtype=np.float32))
    B, Sq, Dm = x.shape
    shared = {}
    for name in ("norm_mix_g", "w_in", "conv_dw_w", "conv_dw_b", "conv_ln_g", "conv_ln_b", "lambda_q1", "lambda_k1",
                 "lambda_q2", "lambda_k2", "diff_subln_g", "w_out", "norm_cross_g", "norm_mem_g", "w_cq", "w_ckv",
                 "w_co", "norm_peer_g", "w_pq", "peer_keys1", "peer_keys2", "peer_u", "peer_v"):
        a = np.asarray(inputs[name], dtype=np.float32)
        shared[name] = np.ascontiguousarray(a[0])
    shared["final_norm_g"] = np.ascontiguousarray(np.asarray(inputs["final_norm_g"], dtype=np.float32))
    in_maps = []
    for c in range(8):
        b, j = c // 4, c % 4
        start = j * 1024
        xwin = np.zeros((NW * 128, Dm), np.float32)
        val = np.zeros((128, NW), np.float32)
        lo = start - 24 * 128
        for w in range(NW):
            p0 = lo + w * 128
            if p0 >= 0:
                xwin[w * 128:(w + 1) * 128] = x[b, p0:p0 + 128]
                val[:, w] = 1.0
        m = dict(shared)
        m["xw"] = xwin
        m["valid"] = val
        m["memb"] = mem[b]
        in_maps.append(m)
    if "nc" not in _NC_CACHE:
        _NC_CACHE["nc"] = build()
    nc = _NC_CACHE["nc"]
    res = run_bass_kernel_spmd(nc, in_maps, core_ids=list(range(8)))
    out = np.zeros((B, Sq, Dm), np.float32)
    for c in range(8):
        b, j = c // 4, c % 4
        out[b, j * 1024:(j + 1) * 1024] = res.results[c]["y"]
    if DBG:
        kernel.dbg = [res.results[c]["dbg"] for c in range(8)]
    return out
```
